# Optimizing a Trainium2 kernel written in Bass

```python
import math
import jax, jax.numpy as jnp
from jax import lax
import numpy as np

D_MODEL = 1024
BATCH = 2
SEQ = 8192
DEPTH = 2

CHUNK = 64
HG_HEADS = 4
HG_DK = 128
HG_DV = 128
HG_KWIDTH = HG_HEADS * HG_DK
HG_VWIDTH = HG_HEADS * HG_DV
GDN_HEADS = 4
GDN_DK = 128
GDN_DV = 128
GDN_QK = GDN_HEADS * GDN_DK
GDN_V = GDN_HEADS * GDN_DV
CONV_K = 4
PEER_HEADS = 8
PEER_DKEY = 256
PEER_NKEYS = 128
PEER_TOPK = 16
PEER_EXPERTS = PEER_NKEYS * PEER_NKEYS
PEER_BLOCK = 128
EPS = 1e-6
IN_SIZES = (HG_KWIDTH, HG_KWIDTH, HG_VWIDTH, HG_VWIDTH, 2 * GDN_QK + GDN_V, GDN_V, GDN_HEADS, GDN_HEADS, D_MODEL, D_MODEL)
IN_COLS = sum(IN_SIZES)

kernel_name = "hybrid_hgrn2_gdn_peer_adaln"


def _offsets(sizes):
    offs, acc = [], 0
    for s in sizes[:-1]:
        acc += s
        offs.append(acc)
    return offs


def rmsnorm(x, g):
    xf = x.astype(jnp.float32)
    y = xf * lax.rsqrt(jnp.mean(xf * xf, axis=-1, keepdims=True) + EPS)
    return (y * g.astype(jnp.float32)).astype(x.dtype)


def l2norm(x):
    return x * lax.rsqrt(jnp.sum(x * x, axis=-1, keepdims=True) + EPS)


def to_chunks(t):
    B, S, H, d = t.shape
    return t.reshape(B, S // CHUNK, CHUNK, H, d).transpose(1, 0, 3, 2, 4)


def from_chunks(t):
    nC, B, H, C, d = t.shape
    return t.transpose(1, 0, 3, 2, 4).reshape(B, nC * C, H, d)


def causal_conv(x, w):
    return lax.conv_general_dilated(x, w[:, None, :], window_strides=(1,), padding=[(CONV_K - 1, 0)],
                                    dimension_numbers=('NWC', 'WIO', 'NWC'), feature_group_count=x.shape[-1])


def hgrn2(q, f, i, g, lb, norm_g):
    B, S, _ = q.shape
    fgate = lb + (1.0 - lb) * jax.nn.sigmoid(f)
    logf = jnp.log(fgate)
    k = 1.0 - fgate
    q = jax.nn.silu(q)
    qc = to_chunks(q.reshape(B, S, HG_HEADS, HG_DK))
    kc = to_chunks(k.reshape(B, S, HG_HEADS, HG_DK))
    lc = to_chunks(logf.reshape(B, S, HG_HEADS, HG_DK))
    vc = to_chunks(i.reshape(B, S, HG_HEADS, HG_DV))
    causal = jnp.tril(jnp.ones((CHUNK, CHUNK), dtype=bool))[:, :, None]

    def step(state, inp):
        qq, kk, ll, vv = inp
        b = jnp.cumsum(ll, axis=2)
        o = jnp.einsum('bhtk,bhkv->bhtv', qq * jnp.exp(b), state)
        diff = jnp.where(causal, b[:, :, :, None, :] - b[:, :, None, :, :], -jnp.inf)
        att = jnp.sum(qq[:, :, :, None, :] * kk[:, :, None, :, :] * jnp.exp(diff), axis=-1)
        o = o + jnp.einsum('bhts,bhsv->bhtv', att, vv)
        b_last = b[:, :, -1:, :]
        state = jnp.exp(b_last[:, :, 0, :])[..., None] * state + jnp.einsum('bhsk,bhsv->bhkv', kk * jnp.exp(b_last - b), vv)
        return state, o

    state0 = jnp.zeros((B, HG_HEADS, HG_DK, HG_DV), jnp.float32)
    _, o = lax.scan(step, state0, (qc, kc, lc, vc))
    o = from_chunks(o)
    o = rmsnorm(o, norm_g) * jax.nn.silu(g.reshape(B, S, HG_HEADS, HG_DV))
    return o.reshape(B, S, HG_VWIDTH)


def gated_deltanet(qkv, g, beta_raw, a_raw, conv_w, a_log, dt_bias, norm_g):
    B, S, _ = qkv.shape
    qkv = jax.nn.silu(causal_conv(qkv, conv_w))
    q, k, v = jnp.split(qkv, [GDN_QK, 2 * GDN_QK], axis=-1)
    q = l2norm(q.reshape(B, S, GDN_HEADS, GDN_DK)) * (GDN_DK ** -0.5)
    k = l2norm(k.reshape(B, S, GDN_HEADS, GDN_DK))
    v = v.reshape(B, S, GDN_HEADS, GDN_DV)
    beta = jax.nn.sigmoid(beta_raw)
    logdec = -jnp.exp(a_log) * jax.nn.softplus(a_raw + dt_bias)
    qc, kc, vc = to_chunks(q), to_chunks(k), to_chunks(v)
    bc = to_chunks(beta[..., None])[..., 0]
    b = jnp.cumsum(to_chunks(logdec[..., None])[..., 0], axis=-1)
    causal = jnp.tril(jnp.ones((CHUNK, CHUNK), dtype=bool))
    strict = jnp.tril(jnp.ones((CHUNK, CHUNK), dtype=bool), k=-1)
    dec = jnp.exp(jnp.where(causal, b[..., :, None] - b[..., None, :], -jnp.inf))
    kb = kc * bc[..., None]
    a_mat = jnp.where(strict, jnp.einsum('nbhid,nbhjd->nbhij', kb, kc) * dec, 0.0)
    u = lax.linalg.triangular_solve(a_mat, vc * bc[..., None], left_side=True, lower=True, unit_diagonal=True)
    w = lax.linalg.triangular_solve(a_mat, kb * jnp.exp(b)[..., None], left_side=True, lower=True, unit_diagonal=True)
    aqk = jnp.einsum('nbhid,nbhjd->nbhij', qc, kc) * dec
    qe = qc * jnp.exp(b)[..., None]
    kd = kc * jnp.exp(b[..., -1:] - b)[..., None]
    b_last = jnp.exp(b[..., -1])

    def step(state, inp):
        uu, ww, aa, qq, kk, bl = inp
        v_new = uu - jnp.einsum('bhck,bhkv->bhcv', ww, state)
        o = jnp.einsum('bhck,bhkv->bhcv', qq, state) + jnp.einsum('bhcs,bhsv->bhcv', aa, v_new)
        state = bl[..., None, None] * state + jnp.einsum('bhsk,bhsv->bhkv', kk, v_new)
        return state, o

    state0 = jnp.zeros((B, GDN_HEADS, GDN_DK, GDN_DV), jnp.float32)
    _, o = lax.scan(step, state0, (u, w, aqk, qe, kd, b_last))
    o = from_chunks(o)
    o = rmsnorm(o, norm_g) * jax.nn.silu(g.reshape(B, S, GDN_HEADS, GDN_DV))
    return o.reshape(B, S, GDN_V)


def token_mixer(h, lb, w_in, hg_norm_g, conv_w, a_log, dt_bias, gdn_norm_g, w_br_hg, w_br_gdn, w_out):
    dt = h.dtype
    proj = (h @ w_in).astype(jnp.float32)
    hq, hf, hi, hg, qkv, gg, beta_raw, a_raw, gate_hg, gate_gdn = jnp.split(proj, _offsets(IN_SIZES), axis=-1)
    y_hg = hgrn2(hq, hf, hi, hg, lb, hg_norm_g)
    y_gdn = gated_deltanet(qkv, gg, beta_raw, a_raw, conv_w.astype(jnp.float32),
                           a_log.astype(jnp.float32), dt_bias.astype(jnp.float32), gdn_norm_g)
    merged = (jax.nn.sigmoid(gate_hg).astype(dt) * (y_hg.astype(dt) @ w_br_hg)
              + jax.nn.sigmoid(gate_gdn).astype(dt) * (y_gdn.astype(dt) @ w_br_gdn))
    return merged @ w_out


def peer(h, wq, subkeys, u_tab, v_tab):
    B, S, D = h.shape
    hb = h.reshape(B * S // PEER_BLOCK, PEER_BLOCK, D)

    def block(xb):
        P = xb.shape[0]
        q = (xb @ wq).reshape(P, PEER_HEADS, 2, PEER_DKEY // 2)
        sc = jnp.einsum('phzd,hznd->phzn', q, subkeys)
        sv, si = lax.top_k(sc, PEER_TOPK)
        cand = (sv[:, :, 0, :, None] + sv[:, :, 1, None, :]).reshape(P, PEER_HEADS, PEER_TOPK * PEER_TOPK)
        cidx = (si[:, :, 0, :, None] * PEER_NKEYS + si[:, :, 1, None, :]).reshape(P, PEER_HEADS, PEER_TOPK * PEER_TOPK)
        top_s, top_c = lax.top_k(cand, PEER_TOPK)
        experts = jnp.take_along_axis(cidx, top_c, axis=-1)
        gate = jax.nn.softmax(top_s.astype(jnp.float32), axis=-1).astype(xb.dtype)
        ue = jnp.take(u_tab, experts, axis=0)
        ve = jnp.take(v_tab, experts, axis=0)
        act = jax.nn.gelu(jnp.einsum('pd,phkd->phk', xb, ue), approximate=False) * gate
        return jnp.einsum('phk,phkd->pd', act, ve)

    return lax.map(block, hb).reshape(B, S, D)


def setup_inputs(seed: int = 0) -> dict:
    key = jax.random.key(seed)
    ks = jax.random.split(key, 24)
    f32 = jnp.float32
    nrm = lambda k, shp, s: (jax.random.normal(k, shp, f32) * s)
    dt = jnp.exp(jax.random.uniform(ks[10], (DEPTH, GDN_HEADS), f32, minval=math.log(1e-3), maxval=math.log(1e-1)))
    return {
        'x': nrm(ks[0], (BATCH, SEQ, D_MODEL), 1.0),
        'c': nrm(ks[1], (BATCH, D_MODEL), 1.0),
        'ada_w': nrm(ks[2], (DEPTH, D_MODEL, 6 * D_MODEL), 0.5 * D_MODEL ** -0.5),
        'ada_b': nrm(ks[3], (DEPTH, 6 * D_MODEL), 0.01),
        'norm1_g': 1.0 + nrm(ks[4], (DEPTH, D_MODEL), 0.02),
        'norm2_g': 1.0 + nrm(ks[5], (DEPTH, D_MODEL), 0.02),
        'final_g': 1.0 + nrm(ks[6], (D_MODEL,), 0.02),
        'w_in': nrm(ks[7], (DEPTH, D_MODEL, IN_COLS), D_MODEL ** -0.5),
        'hg_lb_logits': nrm(ks[8], (DEPTH, HG_KWIDTH), 0.5),
        'hg_norm_g': 1.0 + nrm(ks[9], (DEPTH, HG_DV), 0.02),
        'gdn_conv_w': nrm(ks[11], (DEPTH, CONV_K, 2 * GDN_QK + GDN_V), CONV_K ** -0.5),
        'gdn_a_log': jnp.log(jax.random.uniform(ks[12], (DEPTH, GDN_HEADS), f32, minval=1.0, maxval=16.0)),
        'gdn_dt_bias': dt + jnp.log(-jnp.expm1(-dt)),
        'gdn_norm_g': 1.0 + nrm(ks[13], (DEPTH, GDN_DV), 0.02),
        'w_branch_hg': nrm(ks[14], (DEPTH, HG_VWIDTH, D_MODEL), HG_VWIDTH ** -0.5),
        'w_branch_gdn': nrm(ks[15], (DEPTH, GDN_V, D_MODEL), GDN_V ** -0.5),
        'w_out': nrm(ks[16], (DEPTH, D_MODEL, D_MODEL), D_MODEL ** -0.5),
        'peer_wq': nrm(ks[17], (DEPTH, D_MODEL, PEER_HEADS * PEER_DKEY), D_MODEL ** -0.5),
        'peer_subkeys': nrm(ks[18], (DEPTH, PEER_HEADS, 2, PEER_NKEYS, PEER_DKEY // 2), (PEER_DKEY // 2) ** -0.5),
        'peer_u': nrm(ks[19], (DEPTH, PEER_EXPERTS, D_MODEL), D_MODEL ** -0.5),
        'peer_v': nrm(ks[20], (DEPTH, PEER_EXPERTS, D_MODEL), PEER_HEADS ** -0.5),
    }


def reference(x, c, ada_w, ada_b, norm1_g, norm2_g, final_g, w_in, hg_lb_logits, hg_norm_g,
              gdn_conv_w, gdn_a_log, gdn_dt_bias, gdn_norm_g, w_branch_hg, w_branch_gdn, w_out,
              peer_wq, peer_subkeys, peer_u, peer_v):
    sm = jax.nn.softmax(hg_lb_logits.astype(jnp.float32), axis=0)
    lower_bounds = jnp.cumsum(sm, axis=0) - sm[0:1]
    cond = jax.nn.silu(c)
    for l in range(DEPTH):
        mod = (cond @ ada_w[l] + ada_b[l])[:, None, :]
        sh1, sc1, gt1, sh2, sc2, gt2 = jnp.split(mod, 6, axis=-1)
        h = rmsnorm(x, norm1_g[l]) * (1.0 + sc1) + sh1
        y = token_mixer(h, lower_bounds[l], w_in[l], hg_norm_g[l], gdn_conv_w[l], gdn_a_log[l], gdn_dt_bias[l],
                        gdn_norm_g[l], w_branch_hg[l], w_branch_gdn[l], w_out[l])
        x = x + gt1 * y.astype(x.dtype)
        h = rmsnorm(x, norm2_g[l]) * (1.0 + sc2) + sh2
        x = x + gt2 * peer(h, peer_wq[l], peer_subkeys[l], peer_u[l], peer_v[l]).astype(x.dtype)
    return rmsnorm(x, final_g)
```

```python
import contextlib
import numpy as np
import concourse.bass as bass
import concourse.mybir as mybir
from concourse.bass_utils import run_bass_kernel_spmd

F32 = mybir.dt.float32
BF16 = mybir.dt.bfloat16
I32 = mybir.dt.int32
U32 = mybir.dt.uint32
AF = mybir.ActivationFunctionType
ALU = mybir.AluOpType
AX = mybir.AxisListType

D = 1024
SEQ = 8192
BATCH = 2
DEPTH = 2
EPS = 1e-6
NT_A = SEQ // 128
TOK_B = 2048
NT_B = TOK_B // 128
NEXP = 16384


class Tok:
    __slots__ = ("name", "last_w", "reads", "excl")

    def __init__(self, name="", excl=False):
        self.name = name
        self.last_w = None
        self.reads = {}
        self.excl = excl


class V:
    __slots__ = ("ap", "tok")

    def __init__(self, ap, tok):
        self.ap = ap
        self.tok = tok

    def __getitem__(self, idx):
        return V(self.ap[idx], self.tok)

    def rr(self, pat, **kw):
        return V(self.ap.rearrange(pat, **kw), self.tok)

    def unsq(self, i):
        return V(self.ap.unsqueeze(i), self.tok)

    def bc(self, shape):
        return V(self.ap.broadcast_to(shape), self.tok)

    def bitcast(self, dt):
        return V(self.ap.bitcast(dt), self.tok)


class Prog:
    ENG = ("pe", "act", "dve", "pool", "sp")
    ENGMAP = {"pe": "tensor", "act": "scalar", "dve": "vector", "pool": "gpsimd", "sp": "sync"}

    def __init__(self, nc, n_dma_slots=8):
        self.nc = nc
        self.st = contextlib.ExitStack()
        self.items = {e: [] for e in self.ENG}
        self.count = {}
        self.inc = {}
        for e in ("pe", "act", "dve", "pool"):
            self.count[e] = 0
            self.inc[e] = 1
        self.slots = {}
        self.slot_rr = {}
        for q in ("sp", "pool", "act"):
            names = [f"d_{q}{i}" for i in range(n_dma_slots)]
            self.slots[q] = names
            self.slot_rr[q] = 0
            for n in names:
                self.count[n] = 0
                self.inc[n] = 16
        self.seen = {e: {} for e in self.ENG}
        self.nalloc = 0

    def sb(self, shape, dt, name=None):
        self.nalloc += 1
        name = "sb_" + (name or f"{self.nalloc}")
        h = self.st.enter_context(self.nc.sbuf_tensor(name, list(shape), dt))
        return V(h[:], Tok(name))

    def ps(self, shape, dt, name=None):
        self.nalloc += 1
        name = "ps_" + (name or f"{self.nalloc}")
        h = self.st.enter_context(self.nc.psum_tensor(name, list(shape), dt))
        return V(h[:], Tok(name, excl=True))

    def dram(self, ap, name=""):
        return V(ap, Tok(name))

    def _record(self, stream, tl, fn, reads, writes, extra_waits=()):
        waits = {}
        ex = [t for t in reads if t.excl]
        if ex:
            writes = list(writes) + ex
            reads = [t for t in reads if not t.excl]

        def add(w):
            if w is None:
                return
            t, v = w
            if t == "pe" and stream == "pe":
                return
            if waits.get(t, 0) < v:
                waits[t] = v

        for w in extra_waits:
            add(w)
        pend = getattr(self, "pending", None)
        if pend and pend.get(stream):
            for w in pend.pop(stream):
                add(w)
        for t in reads:
            add(t.last_w)
        for t in writes:
            add(t.last_w)
            for r in t.reads.items():
                add(r)
        seen = self.seen[stream]
        wl = []
        for t, v in waits.items():
            if seen.get(t, 0) >= v:
                continue
            seen[t] = v
            wl.append((t, v))
        self.count[tl] += 1
        val = self.count[tl] * self.inc[tl]
        for t in reads:
            if t.reads.get(tl, 0) < val:
                t.reads[tl] = val
        for t in writes:
            t.last_w = (tl, val)
            t.reads = {}
        self.items[stream].append((wl, fn, tl))

    WKEYS = ("out", "accum_out", "ap")

    def I(self, stream, name, **kw):
        reads, writes, args = [], [], {}
        for k, v in kw.items():
            if isinstance(v, V):
                (writes if k in self.WKEYS else reads).append(v.tok)
                args[k] = v.ap
            else:
                args[k] = v
        if name == "matmul" and kw.get("start") is False:
            pass
        fn = lambda e, name=name, args=args: getattr(e, name)(**args)
        self._record(stream, stream, fn, reads, writes)

    def pe(self, name, **kw):
        self.I("pe", name, **kw)

    def act(self, name, **kw):
        self.I("act", name, **kw)

    def dve(self, name, **kw):
        self.I("dve", name, **kw)

    def pool(self, name, **kw):
        self.I("pool", name, **kw)

    def dma(self, out, in_, q="sp", **kw):
        names = self.slots[q]
        i = self.slot_rr[q]
        self.slot_rr[q] = (i + 1) % len(names)
        tl = names[i]
        extra = []
        if self.count[tl] > 0:
            extra.append((tl, self.count[tl] * 16))
        o, s = out.ap, in_.ap
        fn = lambda e: e.dma_start(out=o, in_=s, **kw)
        self._record(q, tl, fn, [in_.tok], [out.tok], extra)

    def gather(self, out, table, idx):
        q = "pool"
        names = self.slots[q]
        i = self.slot_rr[q]
        self.slot_rr[q] = (i + 1) % len(names)
        tl = names[i]
        extra = []
        if self.count[tl] > 0:
            extra.append((tl, self.count[tl] * 16))
        o, s, ix = out.ap, table.ap, idx.ap
        fn = lambda e: e.indirect_dma_start(out=o, out_offset=None, in_=s,
                                            in_offset=bass.IndirectOffsetOnAxis(ap=ix, axis=0))
        self._record(q, tl, fn, [table.tok, idx.tok], [out.tok], extra)

    def emit(self, final_stream="sp"):
        nc = self.nc
        used = [t for t, c in self.count.items() if c > 0]
        sems = {t: self.st.enter_context(nc.semaphore(f"s_{t}")) for t in used}
        fin = [(t, self.count[t] * self.inc[t]) for t in used]
        block = self.st.enter_context(nc.Block())

        def mk(stream):
            items = self.items[stream]

            def body(eng):
                for wl, fn, tl in items:
                    for t, v in wl:
                        eng.wait_ge(sems[t], v)
                    fn(eng).then_inc(sems[tl], self.inc[tl])
                if stream == final_stream:
                    for t, v in fin:
                        eng.wait_ge(sems[t], v)

            return body

        for stream in self.ENG:
            if self.items[stream] or stream == final_stream:
                getattr(block, self.ENGMAP[stream])(mk(stream))
        self.st.close()
        return nc


C_ID, C_UINC, C_LSTR, C_UBLK, C_LREV, C_ONES, C_CHI, C_USTR, C_UBM, C_MA, C_MB = range(11)
NCONST = 11


def make_consts():
    i = np.arange(128)
    s, t = i[:, None], i[None, :]
    same = (s // 64) == (t // 64)
    c = np.zeros((128, NCONST, 128), np.float32)
    c[:, C_ID] = (s == t)
    c[:, C_UINC] = (s <= t)
    c[:, C_LSTR] = (t < s)
    c[:, C_UBLK] = (s <= t) & same
    c[:, C_LREV] = (s > t) & same
    c[:, C_ONES] = 1.0
    c[:, C_CHI, 0] = (i < 64)
    c[:, C_CHI, 1] = (i >= 64)
    c[:, C_USTR] = (s < t)
    c[:, C_CHI, 2] = (i < 32)
    c[:, C_CHI, 3] = (i >= 64) & (i < 96)
    c[:, C_UBM] = c[:, C_UBLK] - (same & ((s % 64) < 32))
    c[:, C_MA] = ((t % 64) < 32)
    c[:, C_MB] = ((t % 64) >= 32)
    return c


def emit_mod(P, nc, cT_d, adaw_d, adab_d, consts, ncol0, ncols, mod_row, pb, alloc=None):
    alloc = alloc or (lambda shape, dt_, name: P.sb(shape, dt_, name))
    cT = alloc([128, 8], F32, "cT")
    P.dma(cT, cT_d)
    sig = alloc([128, 8], F32, "cTs")
    P.act("activation", out=sig, in_=cT, func=AF.Sigmoid)
    P.dve("tensor_tensor", out=cT, in0=cT, in1=sig, op=ALU.mult)
    brow = alloc([128, ncols], F32, "adab_row")[0:1, :]
    P.dma(brow, adab_d[:, ncol0:ncol0 + ncols])
    wbuf = [alloc([128, 8, 512], F32, f"adaw{i}") for i in range(2)]
    pm = pb[0:1, :]
    for cb in range(ncols // 512):
        wb = wbuf[cb % 2]
        c0 = ncol0 + cb * 512
        P.dma(wb, adaw_d[:, c0:c0 + 512].rr("(j p) n -> p j n", p=128))
        for j in range(8):
            P.pe("matmul", out=pm, lhsT=cT[:, j:j + 1], rhs=wb[:, j, :], start=(j == 0), stop=(j == 7))
        P.dve("tensor_tensor", out=mod_row[:, cb * 512:(cb + 1) * 512], in0=pm, in1=brow[:, cb * 512:(cb + 1) * 512], op=ALU.add)


def emit_bcast_row(P, consts, row, dst, pb):
    n = dst.ap.shape[-1]
    for c0 in range(0, n, 512):
        w = min(512, n - c0)
        P.pe("matmul", out=pb[:, 0:w], lhsT=consts[0:1, C_ONES, :], rhs=row[:, c0:c0 + w], start=True, stop=True)
        P.act("copy", out=dst[:, c0:c0 + w], in_=pb[:, 0:w])


def emit_rstd(P, ss, rstd, n):
    P.dve("tensor_scalar", out=rstd, in0=ss, scalar1=1.0 / n, scalar2=EPS, op0=ALU.mult, op1=ALU.add)
    P.act("activation", out=rstd, in_=rstd, func=AF.Sqrt)
    P.dve("reciprocal", out=rstd, in_=rstd)


class _Stop(Exception):
    pass


def build_A(n_tiles=NT_A, stage=99):
    nc = bass.Bass("TRN2", target_bir_lowering=False)
    S = n_tiles * 128
    dt = lambda name, shape, kind="ExternalInput", d=F32: nc.dram_tensor(name, shape, d, kind=kind).ap()
    x_d = dt("x", [S, D])
    cT_d = dt("cT", [128, 8])
    adaw_d = dt("ada_w", [D, 2048])
    adab_d = dt("ada_b", [1, 2048])
    g1_d = dt("g1", [1, D])
    w_d = dt("w", [D, 1024])
    w2_d = dt("w2", [D, 2])
    lb_d = dt("lbl", [1, 256])
    cols_d = dt("cols", [128, 16])
    rows_d = dt("rows", [1, 256])
    consts_d = dt("consts", [128, NCONST, 128])
    yhg_d = dt("yhg", [S, 128], kind="ExternalOutput")
    ygd_d = dt("ygdn", [S, 128], kind="ExternalOutput")

    P = Prog(nc)
    Dm = lambda ap, n="": P.dram(ap, n)
    x_d, cT_d, adaw_d, adab_d, g1_d, w_d, w2_d, lb_d, cols_d, rows_d, consts_d = [
        Dm(a, f"in{i}") for i, a in enumerate([x_d, cT_d, adaw_d, adab_d, g1_d, w_d, w2_d, lb_d, cols_d, rows_d, consts_d])]
    yhg_d = Dm(yhg_d, "yhg")
    ygd_d = Dm(ygd_d, "ygd")

    consts = P.sb([128, NCONST, 128], F32, "consts")
    P.dma(consts, consts_d)
    ident = consts[:, C_ID, :]
    identb = P.sb([128, 128], BF16, "identb")
    P.dve("tensor_copy", out=identb, in_=ident)
    ones = consts[:, C_ONES, :]

    wsb = P.sb([128, 8, 1024], BF16, "wsb")
    for j in range(8):
        P.dma(wsb[:, j, :], w_d[j * 128:(j + 1) * 128, :], q="pool")
    w2sb = P.sb([128, 8, 2], BF16, "w2sb")
    P.dma(w2sb, w2_d.rr("(j p) n -> p j n", p=128), q="pool")
    cols = P.sb([128, 16], F32, "cols")
    P.dma(cols, cols_d)

    mod_row = P.sb([1, 2048], F32, "mod_row")
    pb = P.ps([128, 512], F32, "ps_bc")
    emit_mod(P, nc, cT_d, adaw_d, adab_d, consts, 0, 2048, mod_row, pb)
    g1row = P.sb([1, D], F32, "g1row")
    P.dma(g1row, g1_d)
    P.dve("scalar_tensor_tensor", out=g1row, in0=mod_row[:, 1024:2048], scalar=1.0, in1=g1row, op0=ALU.add, op1=ALU.mult)
    gs_bc = P.sb([128, D], F32, "gs_bc")
    sh_bc = P.sb([128, D], F32, "sh_bc")
    emit_bcast_row(P, consts, g1row, gs_bc, pb)
    emit_bcast_row(P, consts, mod_row[:, 0:1024], sh_bc, pb)
    rows = P.sb([1, 256], F32, "rows")
    P.dma(rows, rows_d)
    ng_bc = P.sb([128, 256], F32, "ng_bc")
    emit_bcast_row(P, consts, rows, ng_bc, pb)
    lbrow = P.sb([1, 256], F32, "lbrow")
    P.dma(lbrow, lb_d)
    lb_bc2 = P.sb([128, 256], F32, "lb_bc2")
    emit_bcast_row(P, consts, lbrow, lb_bc2, pb)
    lb_bc = P.sb([128, 128], F32, "lb_bc")
    oml_bc = P.sb([128, 128], F32, "oml_bc")
    P.dve("tensor_tensor", out=lb_bc, in0=lb_bc2[:, 128:256], in1=lb_bc2[:, 0:128], op=ALU.subtract)
    P.act("activation", out=lb_bc, in_=lb_bc, func=AF.Sigmoid)
    P.dve("tensor_scalar", out=lb_bc, in0=lb_bc, scalar1=cols[:, 14:15], scalar2=None, op0=ALU.mult)
    P.dve("tensor_scalar", out=oml_bc, in0=lb_bc, scalar1=-1.0, scalar2=1.0, op0=ALU.mult, op1=ALU.add)
    negA = P.sb([128, 1], F32, "negA")
    P.act("activation", out=negA, in_=cols[:, 12:13], func=AF.Exp)
    P.dve("tensor_scalar", out=negA, in0=negA, scalar1=-1.0, scalar2=None, op0=ALU.mult)

    S_hg = P.sb([128, 128], F32, "S_hg")
    S_gd = P.sb([128, 128], F32, "S_gd")
    P.dve("memset", ap=S_hg, constant=0.0)
    P.dve("memset", ap=S_gd, constant=0.0)
    cbuf = P.sb([128, 3, 131], F32, "cbuf")
    P.dve("memset", ap=cbuf, constant=0.0)

    xt = [P.sb([128, D], F32, f"xt{i}") for i in range(2)]
    junk = P.sb([128, D], F32, "junk")
    hb = P.sb([128, D], BF16, "hb")
    hT = P.sb([128, 8, 128], BF16, "hT")
    ss = P.sb([128, 1], F32, "ss")
    rstd = P.sb([128, 1], F32, "rstd")
    ps_tr = P.ps([128, 8, 128], BF16, "ps_tr")
    ps_hg = P.ps([128, 512], F32, "ps_hg")
    ps_gf = P.ps([128, 4, 128], F32, "ps_gf")
    psA = P.ps([128, 512], F32, "psA")
    psB = P.ps([128, 512], F32, "psB")
    psC = P.ps([128, 512], F32, "psC")
    psD = P.ps([128, 512], F32, "psD")

    def t128(name, dt_=F32):
        return P.sb([128, 128], dt_, name)

    q_t, sig_t, fg_t, logf_t, k_t, v_t, gsil_hg = [t128(n) for n in ["q_t", "sig_t", "fg_t", "logf_t", "k_t", "v_t", "gsil_hg"]]
    b_t, brev_t, eb_t, enb_t, ebr_t = [t128(n) for n in ["b_t", "brev_t", "eb_t", "enb_t", "ebr_t"]]
    qe_t, ke_t, kk_t, qeT, keT, attT = [t128(n) for n in ["qe_t", "ke_t", "kk_t", "qeT", "keT", "attT"]]
    dcol = P.sb([128, 4], F32, "dcol")
    keTA, keTB, qeTB, Sp = [t128(n) for n in ["keTA", "keTB", "qeTB", "Sp"]]
    kkm = [t128(f"kkm{i}") for i in range(2)]
    qeTm = [t128(f"qeTm{i}") for i in range(2)]
    P.dve("memset", ap=qeTm[0], constant=0.0)
    P.dve("memset", ap=qeTm[1], constant=0.0)
    yhg_t = [t128(f"yhg_t{i}") for i in range(2)]
    ygd_t = [t128(f"ygd_t{i}") for i in range(2)]
    oss = P.sb([128, 1], F32, "oss")
    orstd = P.sb([128, 1], F32, "orstd")
    cv = P.sb([128, 3, 128], F32, "cv")
    sq3 = P.sb([128, 2, 128], F32, "sq3")
    rn = P.sb([128, 2, 128], F32, "rn")
    qhT, khT, kh_t, vg_t, gsil_gd = [t128(n) for n in ["qhT", "khT", "kh_t", "vg_t", "gsil_gd"]]
    gcol = P.sb([128, 8], F32, "gcol")
    ld_bc, bbc, d1, d2, Nm, Bm, N2, B2, aqkT = [t128(n) for n in ["ld_bc", "bbc", "d1", "d2", "Nm", "Bm", "N2", "B2", "aqkT"]]
    ebbc, qeTg, kd_t, wT, vnew = [t128(n) for n in ["ebbc", "qeTg", "kd_t", "wT", "vnew"]]
    Y = P.sb([128, 256], F32, "Y")
    negb = P.sb([128, 1], F32, "negb")

    try:
      if stage < 2:
        raise _Stop()
      P.dma(xt[0], x_d[0:128, :])
      for it in range(n_tiles):
          xc = xt[it % 2]
          if it + 1 < n_tiles:
              P.dma(xt[(it + 1) % 2], x_d[(it + 1) * 128:(it + 2) * 128, :])
          r0 = it * 128
          P.act("activation", out=junk, in_=xc, func=AF.Square, accum_out=ss)
          emit_rstd(P, ss, rstd, D)
          P.dve("scalar_tensor_tensor", out=junk, in0=xc, scalar=rstd, in1=gs_bc, op0=ALU.mult, op1=ALU.mult)
          P.dve("tensor_tensor", out=hb, in0=junk, in1=sh_bc, op=ALU.add)
          for j in range(8):
              P.pe("transpose", out=ps_tr[:, j, :], in_=hb[:, j * 128:(j + 1) * 128], identity=identb)
          P.act("copy", out=hT, in_=ps_tr)
          for j in range(8):
              P.pe("matmul", out=ps_hg, lhsT=hT[:, j, :], rhs=wsb[:, j, 0:512], start=(j == 0), stop=(j == 7))
          for j in range(8):
              P.pe("matmul", out=ps_gf[:, 3, :], lhsT=hT[:, j, :], rhs=wsb[:, j, 896:1024], start=(j == 0), stop=(j == 7))
          for j in range(8):
              P.pe("matmul", out=pb[:, 0:2], lhsT=hT[:, j, :], rhs=w2sb[:, j, :], start=(j == 0), stop=(j == 7))
          for g in range(3):
              for j in range(8):
                  P.pe("matmul", out=ps_gf[:, g, :], lhsT=wsb[:, j, 512 + g * 128:512 + (g + 1) * 128], rhs=hT[:, j, :],
                       start=(j == 0), stop=(j == 7))
          if stage < 3:
              continue
          P.act("activation", out=q_t, in_=ps_hg[:, 0:128], func=AF.Silu)
          P.act("activation", out=sig_t, in_=ps_hg[:, 128:256], func=AF.Sigmoid)
          P.act("copy", out=v_t, in_=ps_hg[:, 256:384])
          P.act("activation", out=gsil_hg, in_=ps_hg[:, 384:512], func=AF.Silu)
          P.dve("tensor_tensor", out=gsil_hg, in0=gsil_hg, in1=ng_bc[:, 0:128], op=ALU.mult)
          P.dve("tensor_tensor", out=fg_t, in0=sig_t, in1=oml_bc, op=ALU.mult)
          P.dve("tensor_tensor", out=fg_t, in0=fg_t, in1=lb_bc, op=ALU.add)
          P.act("activation", out=logf_t, in_=fg_t, func=AF.Ln)
          P.dve("tensor_scalar", out=k_t, in0=fg_t, scalar1=-1.0, scalar2=1.0, op0=ALU.mult, op1=ALU.add)
          P.pe("matmul", out=psA[:, 0:128], lhsT=consts[:, C_UBM, :], rhs=logf_t, start=True, stop=True)
          P.pe("matmul", out=psA[:, 128:256], lhsT=consts[:, C_LREV, :], rhs=logf_t, start=True, stop=True)
          P.pe("matmul", out=psA[:, 256:260], lhsT=logf_t, rhs=consts[:, C_CHI, 0:4], start=True, stop=True)
          if stage < 3.1:
              continue
          P.act("activation", out=eb_t, in_=psA[:, 0:128], func=AF.Exp)
          P.act("activation", out=enb_t, in_=psA[:, 0:128], func=AF.Exp, scale=-1.0)
          P.act("activation", out=ebr_t, in_=psA[:, 128:256], func=AF.Exp)
          P.act("activation", out=dcol, in_=psA[:, 256:260], func=AF.Exp)
          P.dve("tensor_tensor", out=qe_t, in0=q_t, in1=eb_t, op=ALU.mult)
          P.dve("tensor_tensor", out=ke_t, in0=k_t, in1=enb_t, op=ALU.mult)
          P.dve("tensor_tensor", out=kk_t, in0=k_t, in1=ebr_t, op=ALU.mult)
          if stage < 3.2:
              continue
          P.pe("transpose", out=psB[:, 0:128], in_=qe_t, identity=ident)
          P.pe("transpose", out=psB[:, 128:256], in_=ke_t, identity=ident)
          P.act("copy", out=qeT, in_=psB[:, 0:128])
          P.dve("tensor_copy", out=qeTm[0][:, 0:64], in_=qeT[:, 0:64])
          P.dve("tensor_copy", out=qeTm[1][:, 64:128], in_=qeT[:, 64:128])
          if stage < 3.21:
              continue
          P.dve("tensor_tensor", out=keTA, in0=psB[:, 128:256], in1=consts[:, C_MA, :], op=ALU.mult)
          P.dve("tensor_tensor", out=keTB, in0=psB[:, 128:256], in1=consts[:, C_MB, :], op=ALU.mult)
          P.dve("tensor_tensor", out=qeTB, in0=psB[:, 0:128], in1=consts[:, C_MB, :], op=ALU.mult)
          if stage < 3.22:
              continue
          P.pe("matmul", out=psB[:, 256:384], lhsT=keTA, rhs=qeT, start=True, stop=False)
          P.pe("matmul", out=psB[:, 256:384], lhsT=keTB, rhs=qeTB, start=False, stop=True)
          P.dve("tensor_tensor", out=attT, in0=psB[:, 256:384], in1=consts[:, C_UBLK, :], op=ALU.mult)
          if stage < 3.3:
              continue
          for ch in range(2):
              P.dve("tensor_scalar", out=kkm[ch], in0=kk_t, scalar1=consts[:, C_CHI, ch:ch + 1], scalar2=None, op0=ALU.mult)
          P.dve("tensor_scalar", out=Sp, in0=S_hg, scalar1=dcol[:, 2:3], scalar2=None, op0=ALU.mult)
          P.pe("matmul", out=psC[:, 0:128], lhsT=qeTm[0], rhs=Sp, start=True, stop=False)
          P.pe("matmul", out=psC[:, 0:128], lhsT=attT, rhs=v_t, start=False, stop=False)
          P.pe("matmul", out=psD[:, 0:128], lhsT=kkm[0], rhs=v_t, start=True, stop=True)
          P.dve("scalar_tensor_tensor", out=S_hg, in0=S_hg, scalar=dcol[:, 0:1], in1=psD[:, 0:128], op0=ALU.mult, op1=ALU.add)
          P.dve("tensor_scalar", out=Sp, in0=S_hg, scalar1=dcol[:, 3:4], scalar2=None, op0=ALU.mult)
          P.pe("matmul", out=psC[:, 0:128], lhsT=qeTm[1], rhs=Sp, start=False, stop=True)
          P.pe("matmul", out=psD[:, 128:256], lhsT=kkm[1], rhs=v_t, start=True, stop=True)
          P.dve("scalar_tensor_tensor", out=S_hg, in0=S_hg, scalar=dcol[:, 1:2], in1=psD[:, 128:256], op0=ALU.mult, op1=ALU.add)
          if stage < 3.4:
              continue
          yo = yhg_t[it % 2]
          P.act("activation", out=junk[:, 0:128], in_=psC[:, 0:128], func=AF.Square, accum_out=oss)
          emit_rstd(P, oss, orstd, 128)
          P.dve("scalar_tensor_tensor", out=yo, in0=psC[:, 0:128], scalar=orstd, in1=gsil_hg, op0=ALU.mult, op1=ALU.mult)
          P.dma(yhg_d[r0:r0 + 128, :], yo)

          if stage < 4:
              continue
          P.act("activation", out=gsil_gd, in_=ps_gf[:, 3, :], func=AF.Silu)
          P.dve("tensor_tensor", out=gsil_gd, in0=gsil_gd, in1=ng_bc[:, 128:256], op=ALU.mult)
          P.act("activation", out=gcol[:, 0:1], in_=pb[:, 0:1], func=AF.Sigmoid)
          P.act("activation", out=gcol[:, 1:2], in_=pb[:, 1:2], func=AF.Exp, bias=cols[:, 13:14])
          P.act("activation", out=gcol[:, 1:2], in_=gcol[:, 1:2], func=AF.Ln, bias=1.0)
          P.dve("tensor_scalar", out=gcol[:, 2:3], in0=gcol[:, 1:2], scalar1=negA, scalar2=None, op0=ALU.mult)
          P.dve("tensor_scalar", out=ld_bc, in0=ones, scalar1=gcol[:, 2:3], scalar2=None, op0=ALU.mult)
          P.pe("matmul", out=psA[:, 384:385], lhsT=consts[:, C_UINC, :], rhs=gcol[:, 2:3], start=True, stop=True)
          P.pe("matmul", out=psA[:, 386:387], lhsT=ones, rhs=gcol[:, 2:3], start=True, stop=True)
          P.act("copy", out=gcol[:, 3:4], in_=psA[:, 384:385])
          P.act("copy", out=gcol[:, 4:5], in_=psA[:, 386:387])
          P.pe("matmul", out=psB[:, 384:512], lhsT=ld_bc, rhs=consts[:, C_UINC, :], start=True, stop=True)
          P.act("copy", out=bbc, in_=psB[:, 384:512])
          P.act("activation", out=gcol[:, 5:6], in_=gcol[:, 3:4], func=AF.Exp)
          P.dve("tensor_tensor", out=gcol[:, 6:7], in0=gcol[:, 4:5], in1=gcol[:, 3:4], op=ALU.subtract)
          P.act("activation", out=gcol[:, 6:7], in_=gcol[:, 6:7], func=AF.Exp)
          P.act("activation", out=gcol[:, 7:8], in_=gcol[:, 4:5], func=AF.Exp)
          P.act("activation", out=ebbc, in_=bbc, func=AF.Exp)
          P.dve("tensor_scalar", out=d1, in0=bbc, scalar1=gcol[:, 3:4], scalar2=0.0, op0=ALU.subtract, op1=ALU.max)
          P.dve("tensor_scalar", out=d2, in0=bbc, scalar1=gcol[:, 3:4], scalar2=0.0, op0=ALU.subtract, op1=ALU.min)
          P.act("activation", out=d1, in_=d1, func=AF.Exp, scale=-1.0)
          P.act("activation", out=d2, in_=d2, func=AF.Exp)
          P.dve("tensor_tensor", out=d1, in0=d1, in1=consts[:, C_LSTR, :], op=ALU.mult)
          P.dve("tensor_tensor", out=d2, in0=d2, in1=consts[:, C_UINC, :], op=ALU.mult)
          P.act("copy", out=cbuf[:, :, 3:131], in_=ps_gf[:, 0:3, :])
          for g in range(3):
              P.dve("tensor_scalar", out=cv[:, g, :], in0=cbuf[:, g, 0:128], scalar1=cols[:, g * 4:g * 4 + 1], scalar2=None, op0=ALU.mult)
              for j in range(1, 4):
                  P.dve("scalar_tensor_tensor", out=cv[:, g, :], in0=cbuf[:, g, j:j + 128], scalar=cols[:, g * 4 + j:g * 4 + j + 1],
                        in1=cv[:, g, :], op0=ALU.mult, op1=ALU.add)
          P.dve("tensor_copy", out=cbuf[:, :, 0:3], in_=cbuf[:, :, 128:131])
          P.act("activation", out=cv, in_=cv, func=AF.Silu)
          P.dve("tensor_tensor", out=sq3, in0=cv[:, 0:2, :], in1=cv[:, 0:2, :], op=ALU.mult)
          P.pe("matmul", out=psC[:, 0:256], lhsT=ones, rhs=sq3.rr("p a b -> p (a b)"), start=True, stop=True)
          P.dve("tensor_scalar", out=rn.rr("p a b -> p (a b)"), in0=psC[:, 0:256], scalar1=EPS, scalar2=None, op0=ALU.add)
          P.act("activation", out=rn, in_=rn, func=AF.Sqrt)
          P.dve("reciprocal", out=rn, in_=rn)
          P.dve("scalar_tensor_tensor", out=qhT, in0=cv[:, 0, :], scalar=float(128 ** -0.5), in1=rn[:, 0, :], op0=ALU.mult, op1=ALU.mult)
          P.dve("tensor_tensor", out=khT, in0=cv[:, 1, :], in1=rn[:, 1, :], op=ALU.mult)
          P.pe("transpose", out=psC[:, 256:384], in_=khT, identity=ident)
          P.pe("transpose", out=psC[:, 384:512], in_=cv[:, 2, :], identity=ident)
          P.act("copy", out=kh_t, in_=psC[:, 256:384])
          P.act("copy", out=vg_t, in_=psC[:, 384:512])
          P.pe("matmul", out=psA[:, 0:128], lhsT=khT, rhs=khT, start=True, stop=True)
          P.pe("matmul", out=psA[:, 128:256], lhsT=khT, rhs=qhT, start=True, stop=True)
          P.dve("tensor_scalar", out=negb, in0=gcol[:, 0:1], scalar1=-1.0, scalar2=None, op0=ALU.mult)
          P.dve("scalar_tensor_tensor", out=Nm, in0=psA[:, 0:128], scalar=negb, in1=d1, op0=ALU.mult, op1=ALU.mult)
          P.dve("tensor_tensor", out=aqkT, in0=psA[:, 128:256], in1=d2, op=ALU.mult)
          P.pe("transpose", out=psA[:, 256:384], in_=Nm, identity=ident)
          P.act("copy", out=Bm, in_=psA[:, 256:384])
          P.dve("tensor_scalar", out=Y[:, 0:128], in0=vg_t, scalar1=gcol[:, 0:1], scalar2=None, op0=ALU.mult)
          P.dve("tensor_scalar", out=Y[:, 128:256], in0=kh_t, scalar1=gcol[:, 0:1], scalar2=gcol[:, 5:6], op0=ALU.mult, op1=ALU.mult)
          Ncur, Bcur, Nnxt, Bnxt = Nm, Bm, N2, B2
          for lev in range(7):
              P.pe("matmul", out=psB[:, 0:256], lhsT=Bcur, rhs=Y, start=True, stop=True)
              if lev < 6:
                  P.pe("matmul", out=psC[:, 0:128], lhsT=Bcur, rhs=Ncur, start=True, stop=True)
                  P.pe("matmul", out=psC[:, 128:256], lhsT=Ncur, rhs=Bcur, start=True, stop=True)
              P.dve("tensor_tensor", out=Y, in0=Y, in1=psB[:, 0:256], op=ALU.add)
              if lev < 6:
                  P.act("copy", out=Nnxt, in_=psC[:, 0:128])
                  P.act("copy", out=Bnxt, in_=psC[:, 128:256])
                  Ncur, Bcur, Nnxt, Bnxt = Nnxt, Bnxt, Ncur, Bcur
          P.pe("transpose", out=psA[:, 0:128], in_=Y[:, 128:256], identity=ident)
          P.act("copy", out=wT, in_=psA[:, 0:128])
          P.dve("tensor_tensor", out=qeTg, in0=qhT, in1=ebbc, op=ALU.mult)
          P.dve("tensor_scalar", out=kd_t, in0=kh_t, scalar1=gcol[:, 6:7], scalar2=None, op0=ALU.mult)
          P.pe("matmul", out=psB[:, 256:384], lhsT=wT, rhs=S_gd, start=True, stop=True)
          P.dve("tensor_tensor", out=vnew, in0=Y[:, 0:128], in1=psB[:, 256:384], op=ALU.subtract)
          P.pe("matmul", out=psC[:, 256:384], lhsT=qeTg, rhs=S_gd, start=True, stop=False)
          P.pe("matmul", out=psC[:, 256:384], lhsT=aqkT, rhs=vnew, start=False, stop=True)
          P.pe("matmul", out=psC[:, 384:512], lhsT=kd_t, rhs=vnew, start=True, stop=True)
          P.dve("scalar_tensor_tensor", out=S_gd, in0=S_gd, scalar=gcol[:, 7:8], in1=psC[:, 384:512], op0=ALU.mult, op1=ALU.add)
          yg = ygd_t[it % 2]
          P.act("activation", out=junk[:, 0:128], in_=psC[:, 256:384], func=AF.Square, accum_out=oss)
          emit_rstd(P, oss, orstd, 128)
          P.dve("scalar_tensor_tensor", out=yg, in0=psC[:, 256:384], scalar=orstd, in1=gsil_gd, op0=ALU.mult, op1=ALU.mult)
          P.dma(ygd_d[r0:r0 + 128, :], yg)
    except _Stop:
        pass
    P.emit()
    return nc


_CONSTS = None


def consts_np():
    global _CONSTS
    if _CONSTS is None:
        _CONSTS = make_consts()
    return _CONSTS


def f32c(a):
    return np.ascontiguousarray(a, dtype=np.float32)


def host_inputs_A(inp, x_full, l, b, hd, ntok=SEQ):
    sl = slice(hd * 128, (hd + 1) * 128)
    w_in = inp["w_in"][l]
    offs = [0, 512, 1024, 1536, 2048, 2048 + 512, 2048 + 1024, 3584]
    w = np.concatenate([w_in[:, o + hd * 128:o + (hd + 1) * 128] for o in offs], axis=1)
    w2 = np.stack([w_in[:, 4096 + hd], w_in[:, 4100 + hd]], axis=1)
    cols = np.zeros((128, 16), np.float32)
    cw = inp["gdn_conv_w"][l]
    for g in range(3):
        for j in range(4):
            cols[:, g * 4 + j] = cw[j, g * 512 + hd * 128:g * 512 + (hd + 1) * 128]
    cols[:, 12] = inp["gdn_a_log"][l, hd]
    cols[:, 13] = inp["gdn_dt_bias"][l, hd]
    cols[:, 14] = 0.0 if l == 0 else 1.0
    lg = inp["hg_lb_logits"]
    lbl = np.concatenate([lg[0, sl], lg[1, sl]])[None]
    rows = np.concatenate([inp["hg_norm_g"][l], inp["gdn_norm_g"][l]])[None]
    return {
        "x": f32c(x_full[b, :ntok]),
        "cT": f32c(inp["c"][b].reshape(8, 128).T),
        "ada_w": f32c(inp["ada_w"][l][:, 0:2048]),
        "ada_b": f32c(inp["ada_b"][l][None, 0:2048]),
        "g1": f32c(inp["norm1_g"][l][None]),
        "w": f32c(w),
        "w2": f32c(w2),
        "lbl": f32c(lbl),
        "cols": cols,
        "rows": f32c(rows),
        "consts": consts_np(),
    }


class Arena:
    def __init__(self, P, nfloats, name):
        self.base = P.sb([128, nfloats], F32, name)
        self.n = nfloats
        self.off = 0
        self.k = 0

    def reset(self):
        self.off = 0

    def alloc(self, shape, dt, name=""):
        assert shape[0] == 128
        per = 1
        for s_ in shape[1:]:
            per *= s_
        nf = per if dt in (F32, U32, I32) else (per + 1) // 2
        assert self.off + nf <= self.n, (name, self.off, nf, self.n)
        ap = self.base.ap[:, self.off:self.off + nf]
        self.off += nf
        if dt != F32:
            ap = ap.bitcast(dt)
        if len(shape) == 3:
            ap = ap.rearrange("p (a b) -> p a b", a=shape[1])
        elif len(shape) == 4:
            ap = ap.rearrange("p (a b c) -> p a b c", a=shape[1], b=shape[2])
        self.k += 1
        return V(ap, Tok(f"{name}_{self.k}"))


def _barrier(P):
    allw = [(t, c * P.inc[t]) for t, c in P.count.items() if c > 0]
    P.pending = {e: list(allw) for e in P.ENG}


def build_B(n_tiles=NT_B, final=False, stage=99):
    nc = bass.Bass("TRN2", target_bir_lowering=False)
    T = n_tiles * 128
    dt = lambda name, shape, kind="ExternalInput", d=F32: nc.dram_tensor(name, shape, d, kind=kind).ap()
    P = Prog(nc, n_dma_slots=12)
    Dm = lambda name, shape, **kw: P.dram(dt(name, shape, **kw), name)
    x_d = Dm("x", [T, D])
    yh_d = Dm("yh", [T, 512])
    yg_d = Dm("yg", [T, 512])
    cT_d = Dm("cT", [128, 8])
    adaw_d = Dm("ada_w", [D, 6144])
    adab_d = Dm("ada_b", [1, 6144])
    grow_d = Dm("grow", [1, 3 * D])
    wg_d = Dm("wg", [D, 2048])
    wbh_d = Dm("wbh", [512, D])
    wbg_d = Dm("wbg", [512, D])
    wo_d = Dm("wo", [D, D])
    wq_d = Dm("wq", [D, 2048])
    skT_d = Dm("skT", [128, 16, 128])
    u_d = Dm("u_tab", [NEXP, D])
    v_d = Dm("v_tab", [NEXP, D])
    consts_d = Dm("consts", [128, NCONST, 128])
    iota_d = Dm("iota16", [128, 16])
    xm_d = Dm("xm", [T, D], kind="ExternalOutput")
    xo_d = Dm("xo", [T, D], kind="ExternalOutput")
    u16_d = Dm("u16", [NEXP, D], kind="ExternalOutput", d=BF16)
    v16_d = Dm("v16", [NEXP, D], kind="ExternalOutput", d=BF16)

    consts = P.sb([128, NCONST, 128], F32, "consts")
    P.dma(consts, consts_d)
    ident = consts[:, C_ID, :]
    identb = P.sb([128, 128], BF16, "identb")
    P.dve("tensor_copy", out=identb, in_=ident)
    bcs = P.sb([128, 7, D], F32, "bcs")
    gs1, sh1, gt1, gs2, sh2, gt2, gfin = [bcs[:, i, :] for i in range(7)]
    wbig = P.sb([128, 8, 2048], BF16, "wbig")
    ar = Arena(P, 30 * 1024, "arena")

    pb = P.ps([128, 512], F32, "ps_bc")
    ps_tr = P.ps([128, 8, 128], BF16, "ps_tr")
    psG = [P.ps([128, 512], F32, f"psG{i}") for i in range(2)]
    psQ = [P.ps([128, 4, 128], F32, f"psQ{i}") for i in range(4)]

    P.pending = {}
    for j in range(8):
        P.dma(wbig[:, j, :], wg_d[j * 128:(j + 1) * 128, :], q="pool")
    mod_row = ar.alloc([128, 6144], F32, "mod_row")[0:1, :]
    grow = ar.alloc([128, 3 * D], F32, "grow")[0:1, :]
    emit_mod(P, nc, cT_d, adaw_d, adab_d, consts, 0, 6144, mod_row, pb, alloc=ar.alloc)
    P.dma(grow, grow_d)
    P.dve("scalar_tensor_tensor", out=grow[:, 0:D], in0=mod_row[:, 1024:2048], scalar=1.0, in1=grow[:, 0:D], op0=ALU.add, op1=ALU.mult)
    P.dve("scalar_tensor_tensor", out=grow[:, D:2 * D], in0=mod_row[:, 4096:5120], scalar=1.0, in1=grow[:, D:2 * D], op0=ALU.add, op1=ALU.mult)
    emit_bcast_row(P, consts, grow[:, 0:D], gs1, pb)
    emit_bcast_row(P, consts, mod_row[:, 0:1024], sh1, pb)
    emit_bcast_row(P, consts, mod_row[:, 2048:3072], gt1, pb)
    emit_bcast_row(P, consts, grow[:, D:2 * D], gs2, pb)
    emit_bcast_row(P, consts, mod_row[:, 3072:4096], sh2, pb)
    emit_bcast_row(P, consts, mod_row[:, 5120:6144], gt2, pb)
    emit_bcast_row(P, consts, grow[:, 2 * D:3 * D], gfin, pb)
    _barrier(P)
    ar.reset()

    def norm_mod(xc, gs, sh, hf, hb, junk, ss, rstd):
        P.act("activation", out=junk, in_=xc, func=AF.Square, accum_out=ss)
        emit_rstd(P, ss, rstd, D)
        P.dve("scalar_tensor_tensor", out=junk, in0=xc, scalar=rstd, in1=gs, op0=ALU.mult, op1=ALU.mult)
        if hf is not None:
            P.dve("tensor_tensor", out=hf, in0=junk, in1=sh, op=ALU.add)
            P.act("copy", out=hb, in_=hf)
        else:
            P.dve("tensor_tensor", out=hb, in0=junk, in1=sh, op=ALU.add)

    def transpose_to(hb, hT, nblk):
        for j in range(nblk):
            P.pe("transpose", out=ps_tr[:, j, :], in_=hb[:, j * 128:(j + 1) * 128], identity=identb)
        P.act("copy", out=hT[:, 0:nblk, :], in_=ps_tr[:, 0:nblk, :])

    wbh = ar.alloc([128, 4, D], BF16, "wbh")
    wbg = ar.alloc([128, 4, D], BF16, "wbg")
    wo = ar.alloc([128, 8, D], BF16, "wo")
    P.dma(wbh, wbh_d.rr("(j p) n -> p j n", p=128), q="pool")
    P.dma(wbg, wbg_d.rr("(j p) n -> p j n", p=128), q="pool")
    for j in range(8):
        P.dma(wo[:, j, :], wo_d[j * 128:(j + 1) * 128, :], q="pool")
    xt = [ar.alloc([128, D], F32, f"xt{i}") for i in range(2)]
    junk = ar.alloc([128, D], F32, "junk")
    hb = ar.alloc([128, D], BF16, "hb")
    hT = ar.alloc([128, 8, 128], BF16, "hT")
    gates = ar.alloc([128, 2048], F32, "gates")
    yb = [ar.alloc([128, 512], BF16, f"yb{i}") for i in range(2)]
    yT = [ar.alloc([128, 4, 128], BF16, f"yT{i}") for i in range(2)]
    mg = ar.alloc([128, D], F32, "mg")
    mgb = ar.alloc([128, D], BF16, "mgb")
    mT = ar.alloc([128, 8, 128], BF16, "mT")
    xm = [ar.alloc([128, D], F32, f"xm{i}") for i in range(2)]
    cols = ar.alloc([128, 8], F32, "cols")
    ss, rstd = cols[:, 0:1], cols[:, 1:2]

    P.dma(xt[0], x_d[0:128, :])
    for it in range(n_tiles):
        if stage < 1:
            break
        r0 = it * 128
        xc = xt[it % 2]
        if it + 1 < n_tiles:
            P.dma(xt[(it + 1) % 2], x_d[r0 + 128:r0 + 256, :])
        P.dma(yb[0], yh_d[r0:r0 + 128, :], q="pool")
        P.dma(yb[1], yg_d[r0:r0 + 128, :], q="pool")
        norm_mod(xc, gs1, sh1, None, hb, junk, ss, rstd)
        transpose_to(hb, hT, 8)
        for cb in range(4):
            pg = psG[cb % 2]
            for j in range(8):
                P.pe("matmul", out=pg, lhsT=hT[:, j, :], rhs=wbig[:, j, cb * 512:(cb + 1) * 512], start=(j == 0), stop=(j == 7))
            P.act("activation", out=gates[:, cb * 512:(cb + 1) * 512], in_=pg, func=AF.Sigmoid)
        for br in range(2):
            transpose_to(yb[br], yT[br], 4)
        for cb in range(2):
            cs = slice(cb * 512, (cb + 1) * 512)
            for br, wb in ((0, wbh), (1, wbg)):
                pg = psG[br]
                for j in range(4):
                    P.pe("matmul", out=pg, lhsT=yT[br][:, j, :], rhs=wb[:, j, cs], start=(j == 0), stop=(j == 3))
            P.dve("tensor_tensor", out=mg[:, cs], in0=psG[0], in1=gates[:, cb * 512:(cb + 1) * 512], op=ALU.mult)
            P.dve("tensor_tensor", out=junk[:, cs], in0=psG[1], in1=gates[:, 1024 + cb * 512:1024 + (cb + 1) * 512], op=ALU.mult)
            P.dve("tensor_tensor", out=mgb[:, cs], in0=mg[:, cs], in1=junk[:, cs], op=ALU.add)
        transpose_to(mgb, mT, 8)
        xmc = xm[it % 2]
        for cb in range(2):
            cs = slice(cb * 512, (cb + 1) * 512)
            pg = psG[cb]
            for j in range(8):
                P.pe("matmul", out=pg, lhsT=mT[:, j, :], rhs=wo[:, j, cs], start=(j == 0), stop=(j == 7))
            P.dve("tensor_tensor", out=junk[:, cs], in0=pg, in1=gt1[:, cs], op=ALU.mult)
            P.dve("tensor_tensor", out=xmc[:, cs], in0=junk[:, cs], in1=xc[:, cs], op=ALU.add)
        P.dma(xm_d[r0:r0 + 128, :], xmc)
    _barrier(P)
    ar.reset()

    if stage >= 2:
        for j in range(8):
            P.dma(wbig[:, j, :], wq_d[j * 128:(j + 1) * 128, :], q="pool")
        CH = 1024
        for tb_src, tb_dst in ((u_d, u16_d), (v_d, v16_d)):
            for r in range(0, NEXP, CH):
                P.dma(tb_dst[r:r + CH, :], tb_src[r:r + CH, :], q="pool")
        skT = ar.alloc([128, 16, 128], F32, "skT")
        P.dma(skT, skT_d)
        iota = ar.alloc([128, 16], F32, "iota")
        P.dma(iota, iota_d)
        xt = [ar.alloc([128, D], F32, f"xmt{i}") for i in range(2)]
        junk = ar.alloc([128, D], F32, "junk2")
        h2f = ar.alloc([128, D], F32, "h2f")
        hb = ar.alloc([128, D], BF16, "hb2")
        hT = ar.alloc([128, 8, 128], BF16, "hT2")
        qT = ar.alloc([128, 16, 128], F32, "qT")
        sc = ar.alloc([128, 16, 128], F32, "sc")
        wk = ar.alloc([128, 256], F32, "wk")
        sv = ar.alloc([128, 256], F32, "sv")
        si = ar.alloc([128, 256], U32, "si")
        sif = ar.alloc([128, 256], F32, "sif")
        cand = ar.alloc([128, 2048], F32, "cand")
        oh = ar.alloc([128, 2048], F32, "oh")
        ts = ar.alloc([128, 128], F32, "ts")
        tc = ar.alloc([128, 128], U32, "tc")
        ta = ar.alloc([128, 128], U32, "ta")
        tb = ar.alloc([128, 128], U32, "tb")
        taf = ar.alloc([128, 128], F32, "taf")
        tbf = ar.alloc([128, 128], F32, "tbf")
        i0s = ar.alloc([128, 128], F32, "i0s")
        i1s = ar.alloc([128, 128], F32, "i1s")
        eidx = ar.alloc([128, 128], I32, "eidx")
        gate = ar.alloc([128, 128], F32, "gate")
        actr = ar.alloc([128, 128], F32, "actr")
        actv = ar.alloc([128, 128], F32, "actv")
        cols = ar.alloc([128, 32], F32, "cols2")
        ss, rstd = cols[:, 0:1], cols[:, 1:2]
        gsum = cols[:, 8:16]
        acc = ar.alloc([128, D], F32, "acc")
        NGU, NGV, NDG = 8, 6, 4
        ug = [ar.alloc([128, D], BF16, f"ug{i}") for i in range(NGU)]
        vg = [ar.alloc([128, D], BF16, f"vg{i}") for i in range(NGV)]
        dg = [ar.alloc([128, 128], BF16, f"dg{i}") for i in range(NDG)]
        NPR = 3
        prod = [ar.alloc([128, D], BF16, f"prod{i}") for i in range(NPR)]
        junkb = ar.alloc([128, D], BF16, "junkb")

        P.dma(xt[0], xm_d[0:128, :])
        for it in range(n_tiles):
            r0 = it * 128
            xc = xt[it % 2]
            if it + 1 < n_tiles:
                P.dma(xt[(it + 1) % 2], xm_d[r0 + 128:r0 + 256, :])
            norm_mod(xc, gs2, sh2, h2f, hb, junk, ss, rstd)
            transpose_to(hb, hT, 8)
            for hz in range(16):
                pq = psQ[hz // 4]
                for j in range(8):
                    P.pe("matmul", out=pq[:, hz % 4, :], lhsT=wbig[:, j, hz * 128:(hz + 1) * 128], rhs=hT[:, j, :],
                         start=(j == 0), stop=(j == 7))
                if hz % 4 == 3:
                    P.act("copy", out=qT[:, hz - 3:hz + 1, :], in_=pq)
            for hz in range(16):
                pq = psQ[hz // 4]
                P.pe("matmul", out=pq[:, hz % 4, :], lhsT=qT[:, hz, :], rhs=skT[:, hz, :], start=True, stop=True)
                if hz % 4 == 3:
                    P.act("copy", out=sc[:, hz - 3:hz + 1, :], in_=pq)
            for hz in range(16):
                o8 = hz * 16
                P.dve("max", out=sv[:, o8:o8 + 8], in_=sc[:, hz, :])
                P.dve("max_index", out=si[:, o8:o8 + 8], in_max=sv[:, o8:o8 + 8], in_values=sc[:, hz, :])
                P.dve("match_replace", out=wk[:, 0:128], in_to_replace=sv[:, o8:o8 + 8], in_values=sc[:, hz, :], imm_value=-1e30)
                P.dve("max", out=sv[:, o8 + 8:o8 + 16], in_=wk[:, 0:128])
                P.dve("max_index", out=si[:, o8 + 8:o8 + 16], in_max=sv[:, o8 + 8:o8 + 16], in_values=wk[:, 0:128])
            P.dve("tensor_copy", out=sif, in_=si)
            sv4 = sv.rr("p (h z k) -> p h z k", h=8, z=2)
            sif4 = sif.rr("p (h z k) -> p h z k", h=8, z=2)
            cand4 = cand.rr("p (h a b) -> p h a b", h=8, a=16)
            P.dve("tensor_tensor", out=cand4, in0=sv4[:, :, 0, :].unsq(3).bc([128, 8, 16, 16]),
                  in1=sv4[:, :, 1, :].unsq(2).bc([128, 8, 16, 16]), op=ALU.add)
            for h in range(8):
                o8 = h * 16
                cv_ = cand[:, h * 256:(h + 1) * 256]
                P.dve("max", out=ts[:, o8:o8 + 8], in_=cv_)
                P.dve("max_index", out=tc[:, o8:o8 + 8], in_max=ts[:, o8:o8 + 8], in_values=cv_)
                P.dve("match_replace", out=wk, in_to_replace=ts[:, o8:o8 + 8], in_values=cv_, imm_value=-1e30)
                P.dve("max", out=ts[:, o8 + 8:o8 + 16], in_=wk)
                P.dve("max_index", out=tc[:, o8 + 8:o8 + 16], in_max=ts[:, o8 + 8:o8 + 16], in_values=wk)
            P.dve("tensor_scalar", out=ta, in0=tc, scalar1=4, scalar2=None, op0=ALU.logical_shift_right)
            P.dve("tensor_scalar", out=tb, in0=tc, scalar1=15, scalar2=None, op0=ALU.bitwise_and)
            P.dve("tensor_copy", out=taf, in_=ta)
            P.dve("tensor_copy", out=tbf, in_=tb)
            oh4 = oh.rr("p (h k a) -> p h k a", h=8, k=16)
            io4 = iota.unsq(1).unsq(1).bc([128, 8, 16, 16])
            for z, (tf, isel) in enumerate(((taf, i0s), (tbf, i1s))):
                tf4 = tf.rr("p (h k) -> p h k", h=8).unsq(3).bc([128, 8, 16, 16])
                P.dve("tensor_tensor", out=oh4, in0=tf4, in1=io4, op=ALU.is_equal)
                P.dve("tensor_tensor", out=oh4, in0=oh4, in1=sif4[:, :, z, :].unsq(2).bc([128, 8, 16, 16]), op=ALU.mult)
                P.dve("tensor_reduce", out=isel.rr("p (h k) -> p h k", h=8), in_=oh4, axis=AX.X, op=ALU.add)
            P.dve("scalar_tensor_tensor", out=actr, in0=i0s, scalar=128.0, in1=i1s, op0=ALU.mult, op1=ALU.add)
            P.dve("tensor_copy", out=eidx, in_=actr)
            ts3 = ts.rr("p (h k) -> p h k", h=8)
            g3 = gate.rr("p (h k) -> p h k", h=8)
            P.dve("tensor_tensor", out=g3, in0=ts3, in1=ts3[:, :, 0:1].bc([128, 8, 16]), op=ALU.subtract)
            P.act("activation", out=gate, in_=gate, func=AF.Exp)
            P.dve("tensor_reduce", out=gsum, in_=g3, axis=AX.X, op=ALU.add)
            P.dve("reciprocal", out=gsum, in_=gsum)
            P.dve("tensor_tensor", out=g3, in0=g3, in1=gsum.unsq(2).bc([128, 8, 16]), op=ALU.mult)
            for s_ in range(128):
                ub = ug[s_ % NGU]
                P.gather(ub, u16_d, eidx[:, s_:s_ + 1])
                pr = prod[s_ % NPR]
                P.dve("tensor_tensor", out=pr, in0=ub, in1=hb, op=ALU.mult)
                P.act("activation", out=junkb, in_=pr, func=AF.Copy, accum_out=actr[:, s_:s_ + 1])
            P.act("activation", out=actv, in_=actr, func=AF.Gelu)
            P.dve("tensor_tensor", out=actv, in0=actv, in1=gate, op=ALU.mult)
            for s_ in range(128):
                vb = vg[s_ % NGV]
                dgs = dg[s_ % NDG]
                P.gather(vb, v16_d, eidx[:, s_:s_ + 1])
                P.act("activation", out=dgs, in_=identb, func=AF.Copy, scale=actv[:, s_:s_ + 1])
                for cb in range(2):
                    P.pe("matmul", out=psG[cb], lhsT=dgs, rhs=vb[:, cb * 512:(cb + 1) * 512], start=(s_ == 0), stop=(s_ == 127))
            for cb in range(2):
                cs = slice(cb * 512, (cb + 1) * 512)
                P.dve("tensor_tensor", out=acc[:, cs], in0=psG[cb], in1=gt2[:, cs], op=ALU.mult)
            P.dve("tensor_tensor", out=acc, in0=acc, in1=xc, op=ALU.add)
            if final:
                P.act("activation", out=junk, in_=acc, func=AF.Square, accum_out=ss)
                emit_rstd(P, ss, rstd, D)
                P.dve("scalar_tensor_tensor", out=acc, in0=acc, scalar=rstd, in1=gfin, op0=ALU.mult, op1=ALU.mult)
            P.dma(xo_d[r0:r0 + 128, :], acc)
    P.emit()
    return nc


def host_inputs_B(inp, x_full, yh, yg, l, core, ntok=TOK_B):
    b, seg = core // 4, core % 4
    t0 = seg * TOK_B
    sk = inp["peer_subkeys"][l]
    skT = np.transpose(sk.reshape(16, 128, 128), (2, 0, 1))
    return {
        "x": f32c(x_full[b, t0:t0 + ntok]),
        "yh": f32c(yh[b, t0:t0 + ntok]),
        "yg": f32c(yg[b, t0:t0 + ntok]),
        "cT": f32c(inp["c"][b].reshape(8, 128).T),
        "ada_w": f32c(inp["ada_w"][l]),
        "ada_b": f32c(inp["ada_b"][l][None]),
        "grow": f32c(np.concatenate([inp["norm1_g"][l], inp["norm2_g"][l], inp["final_g"]])[None]),
        "wg": f32c(inp["w_in"][l][:, 4104:6152]),
        "wbh": f32c(inp["w_branch_hg"][l]),
        "wbg": f32c(inp["w_branch_gdn"][l]),
        "wo": f32c(inp["w_out"][l]),
        "wq": f32c(inp["peer_wq"][l]),
        "skT": f32c(skT),
        "u_tab": f32c(inp["peer_u"][l]),
        "v_tab": f32c(inp["peer_v"][l]),
        "consts": consts_np(),
        "iota16": f32c(np.tile(np.arange(16, dtype=np.float32), (128, 1))),
    }


_NC_CACHE = {}


def _get_nc(kind):
    if kind not in _NC_CACHE:
        if kind == "A":
            _NC_CACHE[kind] = build_A(NT_A)
        elif kind == "B":
            _NC_CACHE[kind] = build_B(NT_B, False)
        else:
            _NC_CACHE[kind] = build_B(NT_B, True)
    return _NC_CACHE[kind]


def kernel(**inputs):
    inp = {k: np.asarray(v) for k, v in inputs.items()}
    x_cur = f32c(inp["x"])
    cores = list(range(8))
    for l in range(DEPTH):
        maps = [host_inputs_A(inp, x_cur, l, c // 4, c % 4) for c in cores]
        res = run_bass_kernel_spmd(_get_nc("A"), maps, core_ids=cores).results
        yh = np.empty((BATCH, SEQ, 512), np.float32)
        yg = np.empty((BATCH, SEQ, 512), np.float32)
        for c in cores:
            b, hd = c // 4, c % 4
            yh[b, :, hd * 128:(hd + 1) * 128] = res[c]["yhg"]
            yg[b, :, hd * 128:(hd + 1) * 128] = res[c]["ygdn"]
        maps = [host_inputs_B(inp, x_cur, yh, yg, l, c) for c in cores]
        res = run_bass_kernel_spmd(_get_nc("B" if l < DEPTH - 1 else "Bf"), maps, core_ids=cores).results
        x_new = np.empty((BATCH, SEQ, D), np.float32)
        for c in cores:
            b, seg = c // 4, c % 4
            x_new[b, seg * TOK_B:(seg + 1) * TOK_B] = res[c]["xo"]
        x_cur = x_new
    return x_cur
```

```python
import contextlib
import numpy as np
import concourse.bass as bass
import concourse.mybir as mybir
from concourse.bass_utils import run_bass_kernel_spmd

F32 = mybir.dt.float32
BF16 = mybir.dt.bfloat16
I32 = mybir.dt.int32
U32 = mybir.dt.uint32
AF = mybir.ActivationFunctionType
ALU = mybir.AluOpType
AX = mybir.AxisListType

D = 1024
SEQ = 8192
BATCH = 2
DEPTH = 2
EPS = 1e-6
NT_A = SEQ // 128
TOK_B = 2048
NT_B = TOK_B // 128
NEXP = 16384


class Tok:
    __slots__ = ("name", "last_w", "reads", "excl")

    def __init__(self, name="", excl=False):
        self.name = name
        self.last_w = None
        self.reads = {}
        self.excl = excl


class V:
    __slots__ = ("ap", "tok")

    def __init__(self, ap, tok):
        self.ap = ap
        self.tok = tok

    def __getitem__(self, idx):
        return V(self.ap[idx], self.tok)

    def rr(self, pat, **kw):
        return V(self.ap.rearrange(pat, **kw), self.tok)

    def unsq(self, i):
        return V(self.ap.unsqueeze(i), self.tok)

    def bc(self, shape):
        return V(self.ap.broadcast_to(shape), self.tok)

    def bitcast(self, dt):
        return V(self.ap.bitcast(dt), self.tok)


class Prog:
    ENG = ("pe", "act", "dve", "pool", "sp")
    ENGMAP = {"pe": "tensor", "act": "scalar", "dve": "vector", "pool": "gpsimd", "sp": "sync"}

    def __init__(self, nc, n_dma_slots=8):
        self.nc = nc
        self.st = contextlib.ExitStack()
        self.items = {e: [] for e in self.ENG}
        self.count = {}
        self.inc = {}
        for e in ("pe", "act", "dve", "pool"):
            self.count[e] = 0
            self.inc[e] = 1
        self.slots = {}
        self.slot_rr = {}
        for q in ("sp", "pool", "act"):
            names = [f"d_{q}{i}" for i in range(n_dma_slots)]
            self.slots[q] = names
            self.slot_rr[q] = 0
            for n in names:
                self.count[n] = 0
                self.inc[n] = 16
        self.seen = {e: {} for e in self.ENG}
        self.nalloc = 0

    def sb(self, shape, dt, name=None):
        self.nalloc += 1
        name = "sb_" + (name or f"{self.nalloc}")
        h = self.st.enter_context(self.nc.sbuf_tensor(name, list(shape), dt))
        return V(h[:], Tok(name))

    def ps(self, shape, dt, name=None):
        self.nalloc += 1
        name = "ps_" + (name or f"{self.nalloc}")
        h = self.st.enter_context(self.nc.psum_tensor(name, list(shape), dt))
        return V(h[:], Tok(name, excl=True))

    def dram(self, ap, name=""):
        return V(ap, Tok(name))

    def _record(self, stream, tl, fn, reads, writes, extra_waits=()):
        waits = {}
        ex = [t for t in reads if t.excl]
        if ex:
            writes = list(writes) + ex
            reads = [t for t in reads if not t.excl]

        def add(w):
            if w is None:
                return
            t, v = w
            if t == "pe" and stream == "pe":
                return
            if waits.get(t, 0) < v:
                waits[t] = v

        for w in extra_waits:
            add(w)
        pend = getattr(self, "pending", None)
        if pend and pend.get(stream):
            for w in pend.pop(stream):
                add(w)
        for t in reads:
            add(t.last_w)
        for t in writes:
            add(t.last_w)
            for r in t.reads.items():
                add(r)
        seen = self.seen[stream]
        wl = []
        for t, v in waits.items():
            if seen.get(t, 0) >= v:
                continue
            seen[t] = v
            wl.append((t, v))
        self.count[tl] += 1
        val = self.count[tl] * self.inc[tl]
        for t in reads:
            if t.reads.get(tl, 0) < val:
                t.reads[tl] = val
        for t in writes:
            t.last_w = (tl, val)
            t.reads = {}
        self.items[stream].append((wl, fn, tl))

    WKEYS = ("out", "accum_out", "ap")

    def I(self, stream, name, **kw):
        reads, writes, args = [], [], {}
        for k, v in kw.items():
            if isinstance(v, V):
                (writes if k in self.WKEYS else reads).append(v.tok)
                args[k] = v.ap
            else:
                args[k] = v
        if name == "matmul" and kw.get("start") is False:
            pass
        fn = lambda e, name=name, args=args: getattr(e, name)(**args)
        self._record(stream, stream, fn, reads, writes)

    def pe(self, name, **kw):
        self.I("pe", name, **kw)

    def act(self, name, **kw):
        self.I("act", name, **kw)

    def dve(self, name, **kw):
        self.I("dve", name, **kw)

    def pool(self, name, **kw):
        self.I("pool", name, **kw)

    def dma(self, out, in_, q="sp", **kw):
        names = self.slots[q]
        i = self.slot_rr[q]
        self.slot_rr[q] = (i + 1) % len(names)
        tl = names[i]
        extra = []
        if self.count[tl] > 0:
            extra.append((tl, self.count[tl] * 16))
        o, s = out.ap, in_.ap
        fn = lambda e: e.dma_start(out=o, in_=s, **kw)
        self._record(q, tl, fn, [in_.tok], [out.tok], extra)

    def gather(self, out, table, idx):
        q = "pool"
        names = self.slots[q]
        i = self.slot_rr[q]
        self.slot_rr[q] = (i + 1) % len(names)
        tl = names[i]
        extra = []
        if self.count[tl] > 0:
            extra.append((tl, self.count[tl] * 16))
        o, s, ix = out.ap, table.ap, idx.ap
        fn = lambda e: e.indirect_dma_start(out=o, out_offset=None, in_=s,
                                            in_offset=bass.IndirectOffsetOnAxis(ap=ix, axis=0))
        self._record(q, tl, fn, [table.tok, idx.tok], [out.tok], extra)

    def emit(self, final_stream="sp"):
        nc = self.nc
        used = [t for t, c in self.count.items() if c > 0]
        sems = {t: self.st.enter_context(nc.semaphore(f"s_{t}")) for t in used}
        fin = [(t, self.count[t] * self.inc[t]) for t in used]
        block = self.st.enter_context(nc.Block())

        def mk(stream):
            items = self.items[stream]

            def body(eng):
                for wl, fn, tl in items:
                    for t, v in wl:
                        eng.wait_ge(sems[t], v)
                    fn(eng).then_inc(sems[tl], self.inc[tl])
                if stream == final_stream:
                    for t, v in fin:
                        eng.wait_ge(sems[t], v)

            return body

        for stream in self.ENG:
            if self.items[stream] or stream == final_stream:
                getattr(block, self.ENGMAP[stream])(mk(stream))
        self.st.close()
        return nc


C_ID, C_UINC, C_LSTR, C_UBLK, C_LREV, C_ONES, C_CHI, C_USTR, C_UBM, C_MA, C_MB = range(11)
NCONST = 11


def make_consts():
    i = np.arange(128)
    s, t = i[:, None], i[None, :]
    same = (s // 64) == (t // 64)
    c = np.zeros((128, NCONST, 128), np.float32)
    c[:, C_ID] = (s == t)
    c[:, C_UINC] = (s <= t)
    c[:, C_LSTR] = (t < s)
    c[:, C_UBLK] = (s <= t) & same
    c[:, C_LREV] = (s > t) & same
    c[:, C_ONES] = 1.0
    c[:, C_CHI, 0] = (i < 64)
    c[:, C_CHI, 1] = (i >= 64)
    c[:, C_USTR] = (s < t)
    c[:, C_CHI, 2] = (i < 32)
    c[:, C_CHI, 3] = (i >= 64) & (i < 96)
    c[:, C_UBM] = c[:, C_UBLK] - (same & ((s % 64) < 32))
    c[:, C_MA] = ((t % 64) < 32)
    c[:, C_MB] = ((t % 64) >= 32)
    return c


def emit_mod(P, nc, cT_d, adaw_d, adab_d, consts, ncol0, ncols, mod_row, pb, alloc=None):
    alloc = alloc or (lambda shape, dt_, name: P.sb(shape, dt_, name))
    cT = alloc([128, 8], F32, "cT")
    P.dma(cT, cT_d)
    sig = alloc([128, 8], F32, "cTs")
    P.act("activation", out=sig, in_=cT, func=AF.Sigmoid)
    P.dve("tensor_tensor", out=cT, in0=cT, in1=sig, op=ALU.mult)
    brow = alloc([128, ncols], F32, "adab_row")[0:1, :]
    P.dma(brow, adab_d[:, ncol0:ncol0 + ncols])
    wbuf = [alloc([128, 8, 512], F32, f"adaw{i}") for i in range(2)]
    pm = pb[0:1, :]
    for cb in range(ncols // 512):
        wb = wbuf[cb % 2]
        c0 = ncol0 + cb * 512
        P.dma(wb, adaw_d[:, c0:c0 + 512].rr("(j p) n -> p j n", p=128))
        for j in range(8):
            P.pe("matmul", out=pm, lhsT=cT[:, j:j + 1], rhs=wb[:, j, :], start=(j == 0), stop=(j == 7))
        P.dve("tensor_tensor", out=mod_row[:, cb * 512:(cb + 1) * 512], in0=pm, in1=brow[:, cb * 512:(cb + 1) * 512], op=ALU.add)


def emit_bcast_row(P, consts, row, dst, pb):
    n = dst.ap.shape[-1]
    for c0 in range(0, n, 512):
        w = min(512, n - c0)
        P.pe("matmul", out=pb[:, 0:w], lhsT=consts[0:1, C_ONES, :], rhs=row[:, c0:c0 + w], start=True, stop=True)
        P.act("copy", out=dst[:, c0:c0 + w], in_=pb[:, 0:w])


def emit_rstd(P, ss, rstd, n):
    P.dve("tensor_scalar", out=rstd, in0=ss, scalar1=1.0 / n, scalar2=EPS, op0=ALU.mult, op1=ALU.add)
    P.act("activation", out=rstd, in_=rstd, func=AF.Sqrt)
    P.dve("reciprocal", out=rstd, in_=rstd)


class _Stop(Exception):
    pass


class _Rec:
    def __init__(self):
        self.ops = []

    def pe(self, name, **kw):
        self.ops.append(("pe", name, kw))

    def act(self, name, **kw):
        self.ops.append(("act", name, kw))

    def dve(self, name, **kw):
        self.ops.append(("dve", name, kw))

    def dma(self, out, in_, **kw):
        self.ops.append(("dma", out, in_, kw))


def build_A(n_tiles=NT_A, stage=99):
    nc = bass.Bass("TRN2", target_bir_lowering=False)
    S = n_tiles * 128
    dt = lambda name, shape, kind="ExternalInput", d=F32: nc.dram_tensor(name, shape, d, kind=kind).ap()
    x_d = dt("x", [S, D])
    cT_d = dt("cT", [128, 8])
    adaw_d = dt("ada_w", [D, 2048])
    adab_d = dt("ada_b", [1, 2048])
    g1_d = dt("g1", [1, D])
    w_d = dt("w", [D, 1024])
    w2_d = dt("w2", [D, 2])
    lb_d = dt("lbl", [1, 256])
    cols_d = dt("cols", [128, 16])
    rows_d = dt("rows", [1, 256])
    consts_d = dt("consts", [128, NCONST, 128])
    yhg_d = dt("yhg", [S, 128], kind="ExternalOutput")
    ygd_d = dt("ygdn", [S, 128], kind="ExternalOutput")

    P = Prog(nc)
    Dm = lambda ap, n="": P.dram(ap, n)
    x_d, cT_d, adaw_d, adab_d, g1_d, w_d, w2_d, lb_d, cols_d, rows_d, consts_d = [
        Dm(a, f"in{i}") for i, a in enumerate([x_d, cT_d, adaw_d, adab_d, g1_d, w_d, w2_d, lb_d, cols_d, rows_d, consts_d])]
    yhg_d = Dm(yhg_d, "yhg")
    ygd_d = Dm(ygd_d, "ygd")

    consts = P.sb([128, NCONST, 128], F32, "consts")
    P.dma(consts, consts_d)
    ident = consts[:, C_ID, :]
    identb = P.sb([128, 128], BF16, "identb")
    P.dve("tensor_copy", out=identb, in_=ident)
    ones = consts[:, C_ONES, :]

    wsb = P.sb([128, 8, 1024], BF16, "wsb")
    for j in range(8):
        P.dma(wsb[:, j, :], w_d[j * 128:(j + 1) * 128, :], q="pool")
    w2sb = P.sb([128, 8, 2], BF16, "w2sb")
    P.dma(w2sb, w2_d.rr("(j p) n -> p j n", p=128), q="pool")
    cols = P.sb([128, 16], F32, "cols")
    P.dma(cols, cols_d)

    mod_row = P.sb([1, 2048], F32, "mod_row")
    pb = P.ps([128, 512], F32, "ps_bc")
    emit_mod(P, nc, cT_d, adaw_d, adab_d, consts, 0, 2048, mod_row, pb)
    g1row = P.sb([1, D], F32, "g1row")
    P.dma(g1row, g1_d)
    P.dve("scalar_tensor_tensor", out=g1row, in0=mod_row[:, 1024:2048], scalar=1.0, in1=g1row, op0=ALU.add, op1=ALU.mult)
    gs_bc = P.sb([128, D], F32, "gs_bc")
    sh_bc = P.sb([128, D], F32, "sh_bc")
    emit_bcast_row(P, consts, g1row, gs_bc, pb)
    emit_bcast_row(P, consts, mod_row[:, 0:1024], sh_bc, pb)
    rows = P.sb([1, 256], F32, "rows")
    P.dma(rows, rows_d)
    ng_bc = P.sb([128, 256], F32, "ng_bc")
    emit_bcast_row(P, consts, rows, ng_bc, pb)
    lbrow = P.sb([1, 256], F32, "lbrow")
    P.dma(lbrow, lb_d)
    lb_bc2 = P.sb([128, 256], F32, "lb_bc2")
    emit_bcast_row(P, consts, lbrow, lb_bc2, pb)
    lb_bc = P.sb([128, 128], F32, "lb_bc")
    oml_bc = P.sb([128, 128], F32, "oml_bc")
    P.dve("tensor_tensor", out=lb_bc, in0=lb_bc2[:, 128:256], in1=lb_bc2[:, 0:128], op=ALU.subtract)
    P.act("activation", out=lb_bc, in_=lb_bc, func=AF.Sigmoid)
    P.dve("tensor_scalar", out=lb_bc, in0=lb_bc, scalar1=cols[:, 14:15], scalar2=None, op0=ALU.mult)
    P.dve("tensor_scalar", out=oml_bc, in0=lb_bc, scalar1=-1.0, scalar2=1.0, op0=ALU.mult, op1=ALU.add)
    negA = P.sb([128, 1], F32, "negA")
    P.act("activation", out=negA, in_=cols[:, 12:13], func=AF.Exp)
    P.dve("tensor_scalar", out=negA, in0=negA, scalar1=-1.0, scalar2=None, op0=ALU.mult)

    S_hg = P.sb([128, 128], F32, "S_hg")
    S_gd = P.sb([128, 128], F32, "S_gd")
    P.dve("memset", ap=S_hg, constant=0.0)
    P.dve("memset", ap=S_gd, constant=0.0)
    cbuf = P.sb([128, 3, 131], F32, "cbuf")
    P.dve("memset", ap=cbuf, constant=0.0)

    xt = [P.sb([128, D], F32, f"xt{i}") for i in range(2)]
    junk = P.sb([128, D], F32, "junk")
    hb = P.sb([128, D], BF16, "hb")
    hT = P.sb([128, 8, 128], BF16, "hT")
    ss = P.sb([128, 1], F32, "ss")
    rstd = P.sb([128, 1], F32, "rstd")
    ps_tr = P.ps([128, 8, 128], BF16, "ps_tr")
    ps_hg = P.ps([128, 512], F32, "ps_hg")
    ps_gf = P.ps([128, 4, 128], F32, "ps_gf")
    psA = P.ps([128, 512], F32, "psA")
    psB = P.ps([128, 512], F32, "psB")
    psC = P.ps([128, 512], F32, "psC")
    psD = P.ps([128, 512], F32, "psD")

    def t128(name, dt_=F32):
        return P.sb([128, 128], dt_, name)

    q_t, sig_t, fg_t, logf_t, k_t, v_t, gsil_hg = [t128(n) for n in ["q_t", "sig_t", "fg_t", "logf_t", "k_t", "v_t", "gsil_hg"]]
    b_t, brev_t, eb_t, enb_t, ebr_t = [t128(n) for n in ["b_t", "brev_t", "eb_t", "enb_t", "ebr_t"]]
    qe_t, ke_t, kk_t, qeT, keT, attT = [t128(n) for n in ["qe_t", "ke_t", "kk_t", "qeT", "keT", "attT"]]
    dcol = P.sb([128, 4], F32, "dcol")
    keTA, keTB, qeTB, Sp = [t128(n) for n in ["keTA", "keTB", "qeTB", "Sp"]]
    kkm = [t128(f"kkm{i}") for i in range(2)]
    qeTm = [t128(f"qeTm{i}") for i in range(2)]
    P.dve("memset", ap=qeTm[0], constant=0.0)
    P.dve("memset", ap=qeTm[1], constant=0.0)
    yhg_t = [t128(f"yhg_t{i}") for i in range(2)]
    ygd_t = [t128(f"ygd_t{i}") for i in range(2)]
    oss = P.sb([128, 1], F32, "oss")
    orstd = P.sb([128, 1], F32, "orstd")
    ossg = P.sb([128, 1], F32, "ossg")
    orstdg = P.sb([128, 1], F32, "orstdg")
    junkg = P.sb([128, 128], F32, "junkg")
    cv = P.sb([128, 3, 128], F32, "cv")
    sq3 = P.sb([128, 2, 128], F32, "sq3")
    rn = P.sb([128, 2, 128], F32, "rn")
    qhT, khT, kh_t, vg_t, gsil_gd = [t128(n) for n in ["qhT", "khT", "kh_t", "vg_t", "gsil_gd"]]
    gcol = P.sb([128, 8], F32, "gcol")
    ld_bc, bbc, d1, d2, Nm, Bm, N2, B2, aqkT = [t128(n) for n in ["ld_bc", "bbc", "d1", "d2", "Nm", "Bm", "N2", "B2", "aqkT"]]
    ebbc, qeTg, kd_t, wT, vnew = [t128(n) for n in ["ebbc", "qeTg", "kd_t", "wT", "vnew"]]
    Y = P.sb([128, 256], F32, "Y")
    negb = P.sb([128, 1], F32, "negb")

    try:
      if stage < 2:
        raise _Stop()
      P.dma(xt[0], x_d[0:128, :])
      for it in range(n_tiles):
          xc = xt[it % 2]
          if it + 1 < n_tiles:
              P.dma(xt[(it + 1) % 2], x_d[(it + 1) * 128:(it + 2) * 128, :])
          r0 = it * 128
          P.act("activation", out=junk, in_=xc, func=AF.Square, accum_out=ss)
          emit_rstd(P, ss, rstd, D)
          P.dve("scalar_tensor_tensor", out=junk, in0=xc, scalar=rstd, in1=gs_bc, op0=ALU.mult, op1=ALU.mult)
          P.dve("tensor_tensor", out=hb, in0=junk, in1=sh_bc, op=ALU.add)
          for j in range(8):
              P.pe("transpose", out=ps_tr[:, j, :], in_=hb[:, j * 128:(j + 1) * 128], identity=identb)
          P.act("copy", out=hT, in_=ps_tr)
          for j in range(8):
              P.pe("matmul", out=ps_hg, lhsT=hT[:, j, :], rhs=wsb[:, j, 0:512], start=(j == 0), stop=(j == 7))
          for j in range(8):
              P.pe("matmul", out=ps_gf[:, 3, :], lhsT=hT[:, j, :], rhs=wsb[:, j, 896:1024], start=(j == 0), stop=(j == 7))
          for j in range(8):
              P.pe("matmul", out=pb[:, 0:2], lhsT=hT[:, j, :], rhs=w2sb[:, j, :], start=(j == 0), stop=(j == 7))
          for g in range(3):
              for j in range(8):
                  P.pe("matmul", out=ps_gf[:, g, :], lhsT=wsb[:, j, 512 + g * 128:512 + (g + 1) * 128], rhs=hT[:, j, :],
                       start=(j == 0), stop=(j == 7))
          if stage < 3:
              continue
          PH, PG = _Rec(), _Rec()
          PH.act("activation", out=q_t, in_=ps_hg[:, 0:128], func=AF.Silu)
          PH.act("activation", out=sig_t, in_=ps_hg[:, 128:256], func=AF.Sigmoid)
          PH.act("copy", out=v_t, in_=ps_hg[:, 256:384])
          PH.act("activation", out=gsil_hg, in_=ps_hg[:, 384:512], func=AF.Silu)
          PH.dve("tensor_tensor", out=gsil_hg, in0=gsil_hg, in1=ng_bc[:, 0:128], op=ALU.mult)
          PH.dve("tensor_tensor", out=fg_t, in0=sig_t, in1=oml_bc, op=ALU.mult)
          PH.dve("tensor_tensor", out=fg_t, in0=fg_t, in1=lb_bc, op=ALU.add)
          PH.act("activation", out=logf_t, in_=fg_t, func=AF.Ln)
          PH.dve("tensor_scalar", out=k_t, in0=fg_t, scalar1=-1.0, scalar2=1.0, op0=ALU.mult, op1=ALU.add)
          PH.pe("matmul", out=psA[:, 0:128], lhsT=consts[:, C_UBM, :], rhs=logf_t, start=True, stop=True)
          PH.pe("matmul", out=psA[:, 128:256], lhsT=consts[:, C_LREV, :], rhs=logf_t, start=True, stop=True)
          PH.pe("matmul", out=psA[:, 256:260], lhsT=logf_t, rhs=consts[:, C_CHI, 0:4], start=True, stop=True)
          if stage < 3.1:
              continue
          PH.act("activation", out=eb_t, in_=psA[:, 0:128], func=AF.Exp)
          PH.act("activation", out=enb_t, in_=psA[:, 0:128], func=AF.Exp, scale=-1.0)
          PH.act("activation", out=ebr_t, in_=psA[:, 128:256], func=AF.Exp)
          PH.act("activation", out=dcol, in_=psA[:, 256:260], func=AF.Exp)
          PH.dve("tensor_tensor", out=qe_t, in0=q_t, in1=eb_t, op=ALU.mult)
          PH.dve("tensor_tensor", out=ke_t, in0=k_t, in1=enb_t, op=ALU.mult)
          PH.dve("tensor_tensor", out=kk_t, in0=k_t, in1=ebr_t, op=ALU.mult)
          if stage < 3.2:
              continue
          PH.pe("transpose", out=psB[:, 0:128], in_=qe_t, identity=ident)
          PH.pe("transpose", out=psB[:, 128:256], in_=ke_t, identity=ident)
          PH.act("copy", out=qeT, in_=psB[:, 0:128])
          PH.dve("tensor_copy", out=qeTm[0][:, 0:64], in_=qeT[:, 0:64])
          PH.dve("tensor_copy", out=qeTm[1][:, 64:128], in_=qeT[:, 64:128])
          if stage < 3.21:
              continue
          PH.dve("tensor_tensor", out=keTA, in0=psB[:, 128:256], in1=consts[:, C_MA, :], op=ALU.mult)
          PH.dve("tensor_tensor", out=keTB, in0=psB[:, 128:256], in1=consts[:, C_MB, :], op=ALU.mult)
          PH.dve("tensor_tensor", out=qeTB, in0=psB[:, 0:128], in1=consts[:, C_MB, :], op=ALU.mult)
          if stage < 3.22:
              continue
          PH.pe("matmul", out=psB[:, 256:384], lhsT=keTA, rhs=qeT, start=True, stop=False)
          PH.pe("matmul", out=psB[:, 256:384], lhsT=keTB, rhs=qeTB, start=False, stop=True)
          PH.dve("tensor_tensor", out=attT, in0=psB[:, 256:384], in1=consts[:, C_UBLK, :], op=ALU.mult)
          if stage < 3.3:
              continue
          for ch in range(2):
              PH.dve("tensor_scalar", out=kkm[ch], in0=kk_t, scalar1=consts[:, C_CHI, ch:ch + 1], scalar2=None, op0=ALU.mult)
          PH.dve("tensor_scalar", out=Sp, in0=S_hg, scalar1=dcol[:, 2:3], scalar2=None, op0=ALU.mult)
          PH.pe("matmul", out=psC[:, 0:128], lhsT=qeTm[0], rhs=Sp, start=True, stop=False)
          PH.pe("matmul", out=psC[:, 0:128], lhsT=attT, rhs=v_t, start=False, stop=False)
          PH.pe("matmul", out=psB[:, 384:512], lhsT=kkm[0], rhs=v_t, start=True, stop=True)
          PH.dve("scalar_tensor_tensor", out=S_hg, in0=S_hg, scalar=dcol[:, 0:1], in1=psB[:, 384:512], op0=ALU.mult, op1=ALU.add)
          PH.dve("tensor_scalar", out=Sp, in0=S_hg, scalar1=dcol[:, 3:4], scalar2=None, op0=ALU.mult)
          PH.pe("matmul", out=psC[:, 0:128], lhsT=qeTm[1], rhs=Sp, start=False, stop=True)
          PH.pe("matmul", out=psB[:, 384:512], lhsT=kkm[1], rhs=v_t, start=True, stop=True)
          PH.dve("scalar_tensor_tensor", out=S_hg, in0=S_hg, scalar=dcol[:, 1:2], in1=psB[:, 384:512], op0=ALU.mult, op1=ALU.add)
          if stage < 3.4:
              continue
          yo = yhg_t[it % 2]
          PH.act("activation", out=junk[:, 0:128], in_=psC[:, 0:128], func=AF.Square, accum_out=oss)
          emit_rstd(PH, oss, orstd, 128)
          PH.dve("scalar_tensor_tensor", out=yo, in0=psC[:, 0:128], scalar=orstd, in1=gsil_hg, op0=ALU.mult, op1=ALU.mult)
          PH.dma(yhg_d[r0:r0 + 128, :], yo)

          if stage < 4:
              continue
          PG.act("activation", out=gsil_gd, in_=ps_gf[:, 3, :], func=AF.Silu)
          PG.dve("tensor_tensor", out=gsil_gd, in0=gsil_gd, in1=ng_bc[:, 128:256], op=ALU.mult)
          PG.act("activation", out=gcol[:, 0:1], in_=pb[:, 0:1], func=AF.Sigmoid)
          PG.act("activation", out=gcol[:, 1:2], in_=pb[:, 1:2], func=AF.Exp, bias=cols[:, 13:14])
          PG.act("activation", out=gcol[:, 1:2], in_=gcol[:, 1:2], func=AF.Ln, bias=1.0)
          PG.dve("tensor_scalar", out=gcol[:, 2:3], in0=gcol[:, 1:2], scalar1=negA, scalar2=None, op0=ALU.mult)
          PG.dve("tensor_scalar", out=ld_bc, in0=ones, scalar1=gcol[:, 2:3], scalar2=None, op0=ALU.mult)
          PG.pe("matmul", out=pb[:, 2:3], lhsT=consts[:, C_UINC, :], rhs=gcol[:, 2:3], start=True, stop=True)
          PG.pe("matmul", out=pb[:, 4:5], lhsT=ones, rhs=gcol[:, 2:3], start=True, stop=True)
          PG.act("copy", out=gcol[:, 3:4], in_=pb[:, 2:3])
          PG.act("copy", out=gcol[:, 4:5], in_=pb[:, 4:5])
          PG.pe("matmul", out=pb[:, 128:256], lhsT=ld_bc, rhs=consts[:, C_UINC, :], start=True, stop=True)
          PG.act("copy", out=bbc, in_=pb[:, 128:256])
          PG.act("activation", out=gcol[:, 5:6], in_=gcol[:, 3:4], func=AF.Exp)
          PG.dve("tensor_tensor", out=gcol[:, 6:7], in0=gcol[:, 4:5], in1=gcol[:, 3:4], op=ALU.subtract)
          PG.act("activation", out=gcol[:, 6:7], in_=gcol[:, 6:7], func=AF.Exp)
          PG.act("activation", out=gcol[:, 7:8], in_=gcol[:, 4:5], func=AF.Exp)
          PG.act("activation", out=ebbc, in_=bbc, func=AF.Exp)
          PG.dve("tensor_scalar", out=d1, in0=bbc, scalar1=gcol[:, 3:4], scalar2=0.0, op0=ALU.subtract, op1=ALU.max)
          PG.dve("tensor_scalar", out=d2, in0=bbc, scalar1=gcol[:, 3:4], scalar2=0.0, op0=ALU.subtract, op1=ALU.min)
          PG.act("activation", out=d1, in_=d1, func=AF.Exp, scale=-1.0)
          PG.act("activation", out=d2, in_=d2, func=AF.Exp)
          PG.dve("tensor_tensor", out=d1, in0=d1, in1=consts[:, C_LSTR, :], op=ALU.mult)
          PG.dve("tensor_tensor", out=d2, in0=d2, in1=consts[:, C_UINC, :], op=ALU.mult)
          PG.act("copy", out=cbuf[:, :, 3:131], in_=ps_gf[:, 0:3, :])
          for g in range(3):
              PG.dve("tensor_scalar", out=cv[:, g, :], in0=cbuf[:, g, 0:128], scalar1=cols[:, g * 4:g * 4 + 1], scalar2=None, op0=ALU.mult)
              for j in range(1, 4):
                  PG.dve("scalar_tensor_tensor", out=cv[:, g, :], in0=cbuf[:, g, j:j + 128], scalar=cols[:, g * 4 + j:g * 4 + j + 1],
                        in1=cv[:, g, :], op0=ALU.mult, op1=ALU.add)
          PG.dve("tensor_copy", out=cbuf[:, :, 0:3], in_=cbuf[:, :, 128:131])
          PG.act("activation", out=cv, in_=cv, func=AF.Silu)
          PG.dve("tensor_tensor", out=sq3, in0=cv[:, 0:2, :], in1=cv[:, 0:2, :], op=ALU.mult)
          PG.pe("matmul", out=pb[:, 256:512], lhsT=ones, rhs=sq3.rr("p a b -> p (a b)"), start=True, stop=True)
          PG.dve("tensor_scalar", out=rn.rr("p a b -> p (a b)"), in0=pb[:, 256:512], scalar1=EPS, scalar2=None, op0=ALU.add)
          PG.act("activation", out=rn, in_=rn, func=AF.Sqrt)
          PG.dve("reciprocal", out=rn, in_=rn)
          PG.dve("scalar_tensor_tensor", out=qhT, in0=cv[:, 0, :], scalar=float(128 ** -0.5), in1=rn[:, 0, :], op0=ALU.mult, op1=ALU.mult)
          PG.dve("tensor_tensor", out=khT, in0=cv[:, 1, :], in1=rn[:, 1, :], op=ALU.mult)
          PG.pe("transpose", out=psD[:, 0:128], in_=khT, identity=ident)
          PG.pe("transpose", out=psD[:, 128:256], in_=cv[:, 2, :], identity=ident)
          PG.act("copy", out=kh_t, in_=psD[:, 0:128])
          PG.act("copy", out=vg_t, in_=psD[:, 128:256])
          PG.pe("matmul", out=psD[:, 256:384], lhsT=khT, rhs=khT, start=True, stop=True)
          PG.pe("matmul", out=psD[:, 384:512], lhsT=khT, rhs=qhT, start=True, stop=True)
          PG.dve("tensor_scalar", out=negb, in0=gcol[:, 0:1], scalar1=-1.0, scalar2=None, op0=ALU.mult)
          PG.dve("scalar_tensor_tensor", out=Nm, in0=psD[:, 256:384], scalar=negb, in1=d1, op0=ALU.mult, op1=ALU.mult)
          PG.dve("tensor_tensor", out=aqkT, in0=psD[:, 384:512], in1=d2, op=ALU.mult)
          PG.pe("transpose", out=pb[:, 128:256], in_=Nm, identity=ident)
          PG.act("copy", out=Bm, in_=pb[:, 128:256])
          PG.dve("tensor_scalar", out=Y[:, 0:128], in0=vg_t, scalar1=gcol[:, 0:1], scalar2=None, op0=ALU.mult)
          PG.dve("tensor_scalar", out=Y[:, 128:256], in0=kh_t, scalar1=gcol[:, 0:1], scalar2=gcol[:, 5:6], op0=ALU.mult, op1=ALU.mult)
          Ncur, Bcur, Nnxt, Bnxt = Nm, Bm, N2, B2
          for lev in range(7):
              PG.pe("matmul", out=pb[:, 0:256], lhsT=Bcur, rhs=Y, start=True, stop=True)
              if lev < 6:
                  PG.pe("matmul", out=pb[:, 256:384], lhsT=Bcur, rhs=Ncur, start=True, stop=True)
                  PG.pe("matmul", out=pb[:, 384:512], lhsT=Ncur, rhs=Bcur, start=True, stop=True)
              PG.dve("tensor_tensor", out=Y, in0=Y, in1=pb[:, 0:256], op=ALU.add)
              if lev < 6:
                  PG.act("copy", out=Nnxt, in_=pb[:, 256:384])
                  PG.act("copy", out=Bnxt, in_=pb[:, 384:512])
                  Ncur, Bcur, Nnxt, Bnxt = Nnxt, Bnxt, Ncur, Bcur
          PG.pe("transpose", out=psD[:, 0:128], in_=Y[:, 128:256], identity=ident)
          PG.act("copy", out=wT, in_=psD[:, 0:128])
          PG.dve("tensor_tensor", out=qeTg, in0=qhT, in1=ebbc, op=ALU.mult)
          PG.dve("tensor_scalar", out=kd_t, in0=kh_t, scalar1=gcol[:, 6:7], scalar2=None, op0=ALU.mult)
          PG.pe("matmul", out=psD[:, 128:256], lhsT=wT, rhs=S_gd, start=True, stop=True)
          PG.dve("tensor_tensor", out=vnew, in0=Y[:, 0:128], in1=psD[:, 128:256], op=ALU.subtract)
          PG.pe("matmul", out=psD[:, 256:384], lhsT=qeTg, rhs=S_gd, start=True, stop=False)
          PG.pe("matmul", out=psD[:, 256:384], lhsT=aqkT, rhs=vnew, start=False, stop=True)
          PG.pe("matmul", out=psD[:, 384:512], lhsT=kd_t, rhs=vnew, start=True, stop=True)
          PG.dve("scalar_tensor_tensor", out=S_gd, in0=S_gd, scalar=gcol[:, 7:8], in1=psD[:, 384:512], op0=ALU.mult, op1=ALU.add)
          yg = ygd_t[it % 2]
          PG.act("activation", out=junkg, in_=psD[:, 256:384], func=AF.Square, accum_out=ossg)
          emit_rstd(PG, ossg, orstdg, 128)
          PG.dve("scalar_tensor_tensor", out=yg, in0=psD[:, 256:384], scalar=orstdg, in1=gsil_gd, op0=ALU.mult, op1=ALU.mult)
          PG.dma(ygd_d[r0:r0 + 128, :], yg)
          oh_, og_ = PH.ops, PG.ops
          ih = ig = 0
          while ih < len(oh_) or ig < len(og_):
              if ig >= len(og_) or (ih < len(oh_) and ih * len(og_) <= ig * len(oh_)):
                  op = oh_[ih]; ih += 1
              else:
                  op = og_[ig]; ig += 1
              if op[0] == "dma":
                  P.dma(op[1], op[2], **op[3])
              else:
                  getattr(P, op[0])(op[1], **op[2])
    except _Stop:
        pass
    P.emit()
    return nc


_CONSTS = None


def consts_np():
    global _CONSTS
    if _CONSTS is None:
        _CONSTS = make_consts()
    return _CONSTS


def f32c(a):
    return np.ascontiguousarray(a, dtype=np.float32)


def host_inputs_A(inp, x_full, l, b, hd, ntok=SEQ):
    sl = slice(hd * 128, (hd + 1) * 128)
    w_in = inp["w_in"][l]
    offs = [0, 512, 1024, 1536, 2048, 2048 + 512, 2048 + 1024, 3584]
    w = np.concatenate([w_in[:, o + hd * 128:o + (hd + 1) * 128] for o in offs], axis=1)
    w2 = np.stack([w_in[:, 4096 + hd], w_in[:, 4100 + hd]], axis=1)
    cols = np.zeros((128, 16), np.float32)
    cw = inp["gdn_conv_w"][l]
    for g in range(3):
        for j in range(4):
            cols[:, g * 4 + j] = cw[j, g * 512 + hd * 128:g * 512 + (hd + 1) * 128]
    cols[:, 12] = inp["gdn_a_log"][l, hd]
    cols[:, 13] = inp["gdn_dt_bias"][l, hd]
    cols[:, 14] = 0.0 if l == 0 else 1.0
    lg = inp["hg_lb_logits"]
    lbl = np.concatenate([lg[0, sl], lg[1, sl]])[None]
    rows = np.concatenate([inp["hg_norm_g"][l], inp["gdn_norm_g"][l]])[None]
    return {
        "x": f32c(x_full[b, :ntok]),
        "cT": f32c(inp["c"][b].reshape(8, 128).T),
        "ada_w": f32c(inp["ada_w"][l][:, 0:2048]),
        "ada_b": f32c(inp["ada_b"][l][None, 0:2048]),
        "g1": f32c(inp["norm1_g"][l][None]),
        "w": f32c(w),
        "w2": f32c(w2),
        "lbl": f32c(lbl),
        "cols": cols,
        "rows": f32c(rows),
        "consts": consts_np(),
    }


class Arena:
    def __init__(self, P, nfloats, name):
        self.base = P.sb([128, nfloats], F32, name)
        self.n = nfloats
        self.off = 0
        self.k = 0

    def reset(self):
        self.off = 0

    def alloc(self, shape, dt, name=""):
        assert shape[0] == 128
        per = 1
        for s_ in shape[1:]:
            per *= s_
        nf = per if dt in (F32, U32, I32) else (per + 1) // 2
        assert self.off + nf <= self.n, (name, self.off, nf, self.n)
        ap = self.base.ap[:, self.off:self.off + nf]
        self.off += nf
        if dt != F32:
            ap = ap.bitcast(dt)
        if len(shape) == 3:
            ap = ap.rearrange("p (a b) -> p a b", a=shape[1])
        elif len(shape) == 4:
            ap = ap.rearrange("p (a b c) -> p a b c", a=shape[1], b=shape[2])
        self.k += 1
        return V(ap, Tok(f"{name}_{self.k}"))


def _barrier(P):
    allw = [(t, c * P.inc[t]) for t, c in P.count.items() if c > 0]
    P.pending = {e: list(allw) for e in P.ENG}


def build_B(n_tiles=NT_B, final=False, stage=99):
    nc = bass.Bass("TRN2", target_bir_lowering=False)
    T = n_tiles * 128
    dt = lambda name, shape, kind="ExternalInput", d=F32: nc.dram_tensor(name, shape, d, kind=kind).ap()
    P = Prog(nc, n_dma_slots=12)
    Dm = lambda name, shape, **kw: P.dram(dt(name, shape, **kw), name)
    x_d = Dm("x", [T, D])
    yh_d = Dm("yh", [T, 512])
    yg_d = Dm("yg", [T, 512])
    cT_d = Dm("cT", [128, 8])
    adaw_d = Dm("ada_w", [D, 6144])
    adab_d = Dm("ada_b", [1, 6144])
    grow_d = Dm("grow", [1, 3 * D])
    wg_d = Dm("wg", [D, 2048])
    wbh_d = Dm("wbh", [512, D])
    wbg_d = Dm("wbg", [512, D])
    wo_d = Dm("wo", [D, D])
    wq_d = Dm("wq", [D, 2048])
    skT_d = Dm("skT", [128, 16, 128])
    u_d = Dm("u_tab", [NEXP, D])
    v_d = Dm("v_tab", [NEXP, D])
    consts_d = Dm("consts", [128, NCONST, 128])
    iota_d = Dm("iota16", [128, 16])
    xm_d = Dm("xm", [T, D], kind="ExternalOutput")
    xo_d = Dm("xo", [T, D], kind="ExternalOutput")
    uv16_d = Dm("uv16", [NEXP, 2 * D], kind="ExternalOutput", d=BF16)

    consts = P.sb([128, NCONST, 128], F32, "consts")
    P.dma(consts, consts_d)
    ident = consts[:, C_ID, :]
    identb = P.sb([128, 128], BF16, "identb")
    P.dve("tensor_copy", out=identb, in_=ident)
    bcs = P.sb([128, 7, D], F32, "bcs")
    gs1, sh1, gt1, gs2, sh2, gt2, gfin = [bcs[:, i, :] for i in range(7)]
    wbig = P.sb([128, 8, 2048], BF16, "wbig")
    ar = Arena(P, 30 * 1024, "arena")

    pb = P.ps([128, 512], F32, "ps_bc")
    ps_tr = P.ps([128, 8, 128], BF16, "ps_tr")
    psG = [P.ps([128, 512], F32, f"psG{i}") for i in range(2)]
    psQ = [P.ps([128, 4, 128], F32, f"psQ{i}") for i in range(4)]

    P.pending = {}
    for j in range(8):
        P.dma(wbig[:, j, :], wg_d[j * 128:(j + 1) * 128, :], q="pool")
    mod_row = ar.alloc([128, 6144], F32, "mod_row")[0:1, :]
    grow = ar.alloc([128, 3 * D], F32, "grow")[0:1, :]
    emit_mod(P, nc, cT_d, adaw_d, adab_d, consts, 0, 6144, mod_row, pb, alloc=ar.alloc)
    P.dma(grow, grow_d)
    P.dve("scalar_tensor_tensor", out=grow[:, 0:D], in0=mod_row[:, 1024:2048], scalar=1.0, in1=grow[:, 0:D], op0=ALU.add, op1=ALU.mult)
    P.dve("scalar_tensor_tensor", out=grow[:, D:2 * D], in0=mod_row[:, 4096:5120], scalar=1.0, in1=grow[:, D:2 * D], op0=ALU.add, op1=ALU.mult)
    emit_bcast_row(P, consts, grow[:, 0:D], gs1, pb)
    emit_bcast_row(P, consts, mod_row[:, 0:1024], sh1, pb)
    emit_bcast_row(P, consts, mod_row[:, 2048:3072], gt1, pb)
    emit_bcast_row(P, consts, grow[:, D:2 * D], gs2, pb)
    emit_bcast_row(P, consts, mod_row[:, 3072:4096], sh2, pb)
    emit_bcast_row(P, consts, mod_row[:, 5120:6144], gt2, pb)
    emit_bcast_row(P, consts, grow[:, 2 * D:3 * D], gfin, pb)
    _barrier(P)
    ar.reset()

    def norm_mod(xc, gs, sh, hf, hb, junk, ss, rstd):
        P.act("activation", out=junk, in_=xc, func=AF.Square, accum_out=ss)
        emit_rstd(P, ss, rstd, D)
        P.dve("scalar_tensor_tensor", out=junk, in0=xc, scalar=rstd, in1=gs, op0=ALU.mult, op1=ALU.mult)
        if hf is not None:
            P.dve("tensor_tensor", out=hf, in0=junk, in1=sh, op=ALU.add)
            P.act("copy", out=hb, in_=hf)
        else:
            P.dve("tensor_tensor", out=hb, in0=junk, in1=sh, op=ALU.add)

    def transpose_to(hb, hT, nblk):
        for j in range(nblk):
            P.pe("transpose", out=ps_tr[:, j, :], in_=hb[:, j * 128:(j + 1) * 128], identity=identb)
        P.act("copy", out=hT[:, 0:nblk, :], in_=ps_tr[:, 0:nblk, :])

    wbh = ar.alloc([128, 4, D], BF16, "wbh")
    wbg = ar.alloc([128, 4, D], BF16, "wbg")
    wo = ar.alloc([128, 8, D], BF16, "wo")
    P.dma(wbh, wbh_d.rr("(j p) n -> p j n", p=128), q="pool")
    P.dma(wbg, wbg_d.rr("(j p) n -> p j n", p=128), q="pool")
    for j in range(8):
        P.dma(wo[:, j, :], wo_d[j * 128:(j + 1) * 128, :], q="pool")
    xt = [ar.alloc([128, D], F32, f"xt{i}") for i in range(2)]
    junk = ar.alloc([128, D], F32, "junk")
    hb = ar.alloc([128, D], BF16, "hb")
    hT = ar.alloc([128, 8, 128], BF16, "hT")
    gates = ar.alloc([128, 2048], F32, "gates")
    yb = [ar.alloc([128, 512], BF16, f"yb{i}") for i in range(2)]
    yT = [ar.alloc([128, 4, 128], BF16, f"yT{i}") for i in range(2)]
    mg = ar.alloc([128, D], F32, "mg")
    mgb = ar.alloc([128, D], BF16, "mgb")
    mT = ar.alloc([128, 8, 128], BF16, "mT")
    xm = [ar.alloc([128, D], F32, f"xm{i}") for i in range(2)]
    cols = ar.alloc([128, 8], F32, "cols")
    ss, rstd = cols[:, 0:1], cols[:, 1:2]

    P.dma(xt[0], x_d[0:128, :])
    for it in range(n_tiles):
        if stage < 1:
            break
        r0 = it * 128
        xc = xt[it % 2]
        if it + 1 < n_tiles:
            P.dma(xt[(it + 1) % 2], x_d[r0 + 128:r0 + 256, :])
        P.dma(yb[0], yh_d[r0:r0 + 128, :], q="pool")
        P.dma(yb[1], yg_d[r0:r0 + 128, :], q="pool")
        norm_mod(xc, gs1, sh1, None, hb, junk, ss, rstd)
        transpose_to(hb, hT, 8)
        for cb in range(4):
            pg = psG[cb % 2]
            for j in range(8):
                P.pe("matmul", out=pg, lhsT=hT[:, j, :], rhs=wbig[:, j, cb * 512:(cb + 1) * 512], start=(j == 0), stop=(j == 7))
            P.act("activation", out=gates[:, cb * 512:(cb + 1) * 512], in_=pg, func=AF.Sigmoid)
        for br in range(2):
            transpose_to(yb[br], yT[br], 4)
        for cb in range(2):
            cs = slice(cb * 512, (cb + 1) * 512)
            for br, wb in ((0, wbh), (1, wbg)):
                pg = psG[br]
                for j in range(4):
                    P.pe("matmul", out=pg, lhsT=yT[br][:, j, :], rhs=wb[:, j, cs], start=(j == 0), stop=(j == 3))
            P.dve("tensor_tensor", out=mg[:, cs], in0=psG[0], in1=gates[:, cb * 512:(cb + 1) * 512], op=ALU.mult)
            P.dve("tensor_tensor", out=junk[:, cs], in0=psG[1], in1=gates[:, 1024 + cb * 512:1024 + (cb + 1) * 512], op=ALU.mult)
            P.dve("tensor_tensor", out=mgb[:, cs], in0=mg[:, cs], in1=junk[:, cs], op=ALU.add)
        transpose_to(mgb, mT, 8)
        xmc = xm[it % 2]
        for cb in range(2):
            cs = slice(cb * 512, (cb + 1) * 512)
            pg = psG[cb]
            for j in range(8):
                P.pe("matmul", out=pg, lhsT=mT[:, j, :], rhs=wo[:, j, cs], start=(j == 0), stop=(j == 7))
            P.dve("tensor_tensor", out=junk[:, cs], in0=pg, in1=gt1[:, cs], op=ALU.mult)
            P.dve("tensor_tensor", out=xmc[:, cs], in0=junk[:, cs], in1=xc[:, cs], op=ALU.add)
        P.dma(xm_d[r0:r0 + 128, :], xmc)
    _barrier(P)
    ar.reset()

    if stage >= 2:
        for j in range(8):
            P.dma(wbig[:, j, :], wq_d[j * 128:(j + 1) * 128, :], q="pool")
        CH = 1024
        for half, tb_src in enumerate((u_d, v_d)):
            for r in range(0, NEXP, CH):
                P.dma(uv16_d[r:r + CH, half * D:(half + 1) * D], tb_src[r:r + CH, :], q="pool")
        skT = ar.alloc([128, 16, 128], F32, "skT")
        P.dma(skT, skT_d)
        iota = ar.alloc([128, 16], F32, "iota")
        P.dma(iota, iota_d)
        xt = [ar.alloc([128, D], F32, f"xmt{i}") for i in range(2)]
        junk = ar.alloc([128, D], F32, "junk2")
        h2f = ar.alloc([128, D], F32, "h2f")
        hb = ar.alloc([128, D], BF16, "hb2")
        hT = ar.alloc([128, 8, 128], BF16, "hT2")
        qT = ar.alloc([128, 16, 128], F32, "qT")
        sc = ar.alloc([128, 16, 128], F32, "sc")
        wk = ar.alloc([128, 256], F32, "wk")
        sv = ar.alloc([128, 256], F32, "sv")
        si = ar.alloc([128, 256], U32, "si")
        sif = ar.alloc([128, 256], F32, "sif")
        cand = ar.alloc([128, 2048], F32, "cand")
        oh = ar.alloc([128, 2048], F32, "oh")
        ts = ar.alloc([128, 128], F32, "ts")
        tc = ar.alloc([128, 128], U32, "tc")
        ta = ar.alloc([128, 128], U32, "ta")
        tb = ar.alloc([128, 128], U32, "tb")
        taf = ar.alloc([128, 128], F32, "taf")
        tbf = ar.alloc([128, 128], F32, "tbf")
        i0s = ar.alloc([128, 128], F32, "i0s")
        i1s = ar.alloc([128, 128], F32, "i1s")
        eidx = ar.alloc([128, 128], I32, "eidx")
        gate = ar.alloc([128, 128], F32, "gate")
        actr = ar.alloc([128, 128], F32, "actr")
        actv = ar.alloc([128, 128], F32, "actv")
        cols = ar.alloc([128, 32], F32, "cols2")
        ss, rstd = cols[:, 0:1], cols[:, 1:2]
        gsum = cols[:, 8:16]
        acc = ar.alloc([128, D], F32, "acc")
        NGUV, NDG, GRP = 8, 4, 4
        uvg = [ar.alloc([128, 2 * D], BF16, f"uvg{i}") for i in range(NGUV)]
        dg = [ar.alloc([128, 128], BF16, f"dg{i}") for i in range(NDG)]

        P.dma(xt[0], xm_d[0:128, :])
        for it in range(n_tiles):
            r0 = it * 128
            xc = xt[it % 2]
            if it + 1 < n_tiles:
                P.dma(xt[(it + 1) % 2], xm_d[r0 + 128:r0 + 256, :])
            norm_mod(xc, gs2, sh2, h2f, hb, junk, ss, rstd)
            transpose_to(hb, hT, 8)
            for hz in range(16):
                pq = psQ[hz // 4]
                for j in range(8):
                    P.pe("matmul", out=pq[:, hz % 4, :], lhsT=wbig[:, j, hz * 128:(hz + 1) * 128], rhs=hT[:, j, :],
                         start=(j == 0), stop=(j == 7))
                if hz % 4 == 3:
                    P.act("copy", out=qT[:, hz - 3:hz + 1, :], in_=pq)
            for hz in range(16):
                pq = psQ[hz // 4]
                P.pe("matmul", out=pq[:, hz % 4, :], lhsT=qT[:, hz, :], rhs=skT[:, hz, :], start=True, stop=True)
                if hz % 4 == 3:
                    P.act("copy", out=sc[:, hz - 3:hz + 1, :], in_=pq)
            for hz in range(16):
                o8 = hz * 16
                P.dve("max", out=sv[:, o8:o8 + 8], in_=sc[:, hz, :])
                P.dve("max_index", out=si[:, o8:o8 + 8], in_max=sv[:, o8:o8 + 8], in_values=sc[:, hz, :])
                P.dve("match_replace", out=wk[:, 0:128], in_to_replace=sv[:, o8:o8 + 8], in_values=sc[:, hz, :], imm_value=-1e30)
                P.dve("max", out=sv[:, o8 + 8:o8 + 16], in_=wk[:, 0:128])
                P.dve("max_index", out=si[:, o8 + 8:o8 + 16], in_max=sv[:, o8 + 8:o8 + 16], in_values=wk[:, 0:128])
            P.dve("tensor_copy", out=sif, in_=si)
            sv4 = sv.rr("p (h z k) -> p h z k", h=8, z=2)
            sif4 = sif.rr("p (h z k) -> p h z k", h=8, z=2)
            cand4 = cand.rr("p (h a b) -> p h a b", h=8, a=16)
            P.dve("tensor_tensor", out=cand4, in0=sv4[:, :, 0, :].unsq(3).bc([128, 8, 16, 16]),
                  in1=sv4[:, :, 1, :].unsq(2).bc([128, 8, 16, 16]), op=ALU.add)
            for h in range(8):
                o8 = h * 16
                cv_ = cand[:, h * 256:(h + 1) * 256]
                P.dve("max", out=ts[:, o8:o8 + 8], in_=cv_)
                P.dve("max_index", out=tc[:, o8:o8 + 8], in_max=ts[:, o8:o8 + 8], in_values=cv_)
                P.dve("match_replace", out=wk, in_to_replace=ts[:, o8:o8 + 8], in_values=cv_, imm_value=-1e30)
                P.dve("max", out=ts[:, o8 + 8:o8 + 16], in_=wk)
                P.dve("max_index", out=tc[:, o8 + 8:o8 + 16], in_max=ts[:, o8 + 8:o8 + 16], in_values=wk)
            P.dve("tensor_scalar", out=ta, in0=tc, scalar1=4, scalar2=None, op0=ALU.logical_shift_right)
            P.dve("tensor_scalar", out=tb, in0=tc, scalar1=15, scalar2=None, op0=ALU.bitwise_and)
            P.dve("tensor_copy", out=taf, in_=ta)
            P.dve("tensor_copy", out=tbf, in_=tb)
            oh4 = oh.rr("p (h k a) -> p h k a", h=8, k=16)
            io4 = iota.unsq(1).unsq(1).bc([128, 8, 16, 16])
            for z, (tf, isel) in enumerate(((taf, i0s), (tbf, i1s))):
                tf4 = tf.rr("p (h k) -> p h k", h=8).unsq(3).bc([128, 8, 16, 16])
                P.dve("tensor_tensor", out=oh4, in0=tf4, in1=io4, op=ALU.is_equal)
                P.dve("tensor_tensor", out=oh4, in0=oh4, in1=sif4[:, :, z, :].unsq(2).bc([128, 8, 16, 16]), op=ALU.mult)
                P.dve("tensor_reduce", out=isel.rr("p (h k) -> p h k", h=8), in_=oh4, axis=AX.X, op=ALU.add)
            P.dve("scalar_tensor_tensor", out=actr, in0=i0s, scalar=128.0, in1=i1s, op0=ALU.mult, op1=ALU.add)
            P.dve("tensor_copy", out=eidx, in_=actr)
            ts3 = ts.rr("p (h k) -> p h k", h=8)
            g3 = gate.rr("p (h k) -> p h k", h=8)
            P.dve("tensor_tensor", out=g3, in0=ts3, in1=ts3[:, :, 0:1].bc([128, 8, 16]), op=ALU.subtract)
            P.act("activation", out=gate, in_=gate, func=AF.Exp)
            P.dve("tensor_reduce", out=gsum, in_=g3, axis=AX.X, op=ALU.add)
            P.dve("reciprocal", out=gsum, in_=gsum)
            P.dve("tensor_tensor", out=g3, in0=g3, in1=gsum.unsq(2).bc([128, 8, 16]), op=ALU.mult)
            for g0 in range(0, 128, GRP):
                for s_ in range(g0, g0 + GRP):
                    ub = uvg[s_ % NGUV]
                    P.gather(ub, uv16_d, eidx[:, s_:s_ + 1])
                    P.dve("scalar_tensor_tensor", out=junk, in0=ub[:, 0:D], scalar=1.0, in1=h2f, op0=ALU.mult, op1=ALU.mult,
                          accum_out=actr[:, s_:s_ + 1])
                P.act("activation", out=actv[:, g0:g0 + GRP], in_=actr[:, g0:g0 + GRP], func=AF.Gelu)
                P.dve("tensor_tensor", out=actv[:, g0:g0 + GRP], in0=actv[:, g0:g0 + GRP], in1=gate[:, g0:g0 + GRP], op=ALU.mult)
                for s_ in range(g0, g0 + GRP):
                    ub = uvg[s_ % NGUV]
                    dgs = dg[s_ % NDG]
                    P.dve("tensor_scalar", out=dgs, in0=identb, scalar1=actv[:, s_:s_ + 1], scalar2=None, op0=ALU.mult)
                    for cb in range(2):
                        P.pe("matmul", out=psG[cb], lhsT=dgs, rhs=ub[:, D + cb * 512:D + (cb + 1) * 512], start=(s_ == 0), stop=(s_ == 127))
            for cb in range(2):
                cs = slice(cb * 512, (cb + 1) * 512)
                P.dve("tensor_tensor", out=acc[:, cs], in0=psG[cb], in1=gt2[:, cs], op=ALU.mult)
            P.dve("tensor_tensor", out=acc, in0=acc, in1=xc, op=ALU.add)
            if final:
                P.act("activation", out=junk, in_=acc, func=AF.Square, accum_out=ss)
                emit_rstd(P, ss, rstd, D)
                P.dve("scalar_tensor_tensor", out=acc, in0=acc, scalar=rstd, in1=gfin, op0=ALU.mult, op1=ALU.mult)
            P.dma(xo_d[r0:r0 + 128, :], acc)
    P.emit()
    return nc


def host_inputs_B(inp, x_full, yh, yg, l, core, ntok=TOK_B):
    b, seg = core // 4, core % 4
    t0 = seg * TOK_B
    sk = inp["peer_subkeys"][l]
    skT = np.transpose(sk.reshape(16, 128, 128), (2, 0, 1))
    return {
        "x": f32c(x_full[b, t0:t0 + ntok]),
        "yh": f32c(yh[b, t0:t0 + ntok]),
        "yg": f32c(yg[b, t0:t0 + ntok]),
        "cT": f32c(inp["c"][b].reshape(8, 128).T),
        "ada_w": f32c(inp["ada_w"][l]),
        "ada_b": f32c(inp["ada_b"][l][None]),
        "grow": f32c(np.concatenate([inp["norm1_g"][l], inp["norm2_g"][l], inp["final_g"]])[None]),
        "wg": f32c(inp["w_in"][l][:, 4104:6152]),
        "wbh": f32c(inp["w_branch_hg"][l]),
        "wbg": f32c(inp["w_branch_gdn"][l]),
        "wo": f32c(inp["w_out"][l]),
        "wq": f32c(inp["peer_wq"][l]),
        "skT": f32c(skT),
        "u_tab": f32c(inp["peer_u"][l]),
        "v_tab": f32c(inp["peer_v"][l]),
        "consts": consts_np(),
        "iota16": f32c(np.tile(np.arange(16, dtype=np.float32), (128, 1))),
    }


_NC_CACHE = {}


def _get_nc(kind):
    if kind not in _NC_CACHE:
        if kind == "A":
            _NC_CACHE[kind] = build_A(NT_A)
        elif kind == "B":
            _NC_CACHE[kind] = build_B(NT_B, False)
        else:
            _NC_CACHE[kind] = build_B(NT_B, True)
    return _NC_CACHE[kind]


def kernel(**inputs):
    inp = {k: np.asarray(v) for k, v in inputs.items()}
    x_cur = f32c(inp["x"])
    cores = list(range(8))
    for l in range(DEPTH):
        maps = [host_inputs_A(inp, x_cur, l, c // 4, c % 4) for c in cores]
        res = run_bass_kernel_spmd(_get_nc("A"), maps, core_ids=cores).results
        yh = np.empty((BATCH, SEQ, 512), np.float32)
        yg = np.empty((BATCH, SEQ, 512), np.float32)
        for c in cores:
            b, hd = c // 4, c % 4
            yh[b, :, hd * 128:(hd + 1) * 128] = res[c]["yhg"]
            yg[b, :, hd * 128:(hd + 1) * 128] = res[c]["ygdn"]
        maps = [host_inputs_B(inp, x_cur, yh, yg, l, c) for c in cores]
        res = run_bass_kernel_spmd(_get_nc("B" if l < DEPTH - 1 else "Bf"), maps, core_ids=cores).results
        x_new = np.empty((BATCH, SEQ, D), np.float32)
        for c in cores:
            b, seg = c // 4, c % 4
            x_new[b, seg * TOK_B:(seg + 1) * TOK_B] = res[c]["xo"]
        x_cur = x_new
    return x_cur
```
